# Optimizing a Trainium2 kernel written in Bass

```python
import math
import jax
import jax.numpy as jnp
from jax import lax
import numpy as np

D_MODEL = 1024
BATCH = 8
SEQ = 2048
DEPTH = 4

CHUNK = 64
Q_BLOCK = 128
N_MIXERS = 4
D_MIX = D_MODEL
GROUP_WIDTH = D_MIX // N_MIXERS
N_HEADS = 4
HEAD_DIM = GROUP_WIDTH // N_HEADS
D_FF = 11 * D_MODEL // 4
CONV_K = 4
SSD_GROUPS = 2
SSD_STATE = 64
ROPE_BASE = 10000.0
RET_DECAY_OFFSET = 5.0
RMS_EPS = 1e-6
N_MOD = 9
FOX_COLS = 3 * GROUP_WIDTH + N_HEADS
MLSTM_COLS = 4 * GROUP_WIDTH + 2 * N_HEADS
RET_COLS = 4 * GROUP_WIDTH
SSD_COLS = 2 * GROUP_WIDTH + 2 * SSD_GROUPS * SSD_STATE + N_HEADS
D_IN_PROJ = FOX_COLS + MLSTM_COLS + RET_COLS + SSD_COLS

kernel_name = 'hybrid_chunk_causal_encoder'


def rmsnorm(x, g):
    xf = x.astype(jnp.float32)
    y = xf * lax.rsqrt(jnp.mean(xf * xf, axis=-1, keepdims=True) + RMS_EPS)
    return (y * g.astype(jnp.float32)).astype(x.dtype)


def head_rmsnorm(y, g):
    _, H, _, d = y.shape
    y = y * lax.rsqrt(jnp.mean(y * y, axis=-1, keepdims=True) + RMS_EPS)
    return y * g.astype(jnp.float32).reshape(H, 1, d)


def modulate(h, shift, scale):
    return h * (1 + scale) + shift


def swiglu(u, w13, w2):
    a, g = jnp.split(u @ w13, 2, axis=-1)
    return (jax.nn.silu(g) * a) @ w2


def to_heads(t):
    B, S, _ = t.shape
    return t.reshape(B, S, N_HEADS, -1).transpose(0, 2, 1, 3)


def merge_heads(t):
    B, H, S, d = t.shape
    return t.transpose(0, 2, 1, 3).reshape(B, S, H * d)


def causal_dwconv(x, w, b):
    C = x.shape[-1]
    y = lax.conv_general_dilated(
        x, w[:, None, :].astype(x.dtype), window_strides=(1,),
        padding=[(CONV_K - 1, 0)], dimension_numbers=('NWC', 'WIO', 'NWC'),
        feature_group_count=C)
    return y + b


def rotary(t):
    S, d = t.shape[2], t.shape[3]
    inv = 1.0 / (ROPE_BASE ** (jnp.arange(0, d, 2, dtype=jnp.float32) / d))
    ang = jnp.arange(S, dtype=jnp.float32)[:, None] * inv[None, :]
    cos, sin = jnp.cos(ang), jnp.sin(ang)
    t1, t2 = t[..., : d // 2], t[..., d // 2:]
    return jnp.concatenate([t1 * cos - t2 * sin, t1 * sin + t2 * cos], axis=-1)


def forgetting_attention(q, k, v, log_f):
    S, d = q.shape[2], q.shape[3]
    cum = jnp.cumsum(log_f, axis=-1)
    scale = d ** -0.5
    outs = []
    for blk in range(S // Q_BLOCK):
        lo, hi = blk * Q_BLOCK, (blk + 1) * Q_BLOCK
        logits = (jnp.einsum('bhtd,bhsd->bhts', q[:, :, lo:hi], k[:, :, :hi]) * scale
                  + cum[:, :, lo:hi, None] - cum[:, :, None, :hi])
        causal = (lo + jnp.arange(Q_BLOCK))[:, None] >= jnp.arange(hi)[None, :]
        p = jax.nn.softmax(jnp.where(causal, logits, -jnp.inf), axis=-1)
        outs.append(jnp.einsum('bhts,bhsd->bhtd', p, v[:, :, :hi]))
    return jnp.concatenate(outs, axis=2)


def chunked_decay_attention(q, k, v, log_a):
    B, H, S, dk = q.shape
    dv = v.shape[-1]
    n = S // CHUNK
    qc = q.reshape(B, H, n, CHUNK, dk)
    kc = k.reshape(B, H, n, CHUNK, dk)
    vc = v.reshape(B, H, n, CHUNK, dv)
    cum = jnp.cumsum(log_a.reshape(B, H, n, CHUNK), axis=-1)
    causal = jnp.tril(jnp.ones((CHUNK, CHUNK), dtype=bool))
    decay = jnp.exp(jnp.where(causal, cum[..., :, None] - cum[..., None, :], -jnp.inf))
    scores = jnp.einsum('bhntd,bhnsd->bhnts', qc, kc) * decay
    y = jnp.einsum('bhnts,bhnse->bhnte', scores, vc)
    w_end = jnp.exp(cum[..., -1:] - cum)
    kv = jnp.einsum('bhnsd,bhns,bhnse->nbhde', kc, w_end, vc)
    a_chunk = jnp.exp(cum[..., -1]).transpose(2, 0, 1)

    def step(state, inp):
        a, kv_c = inp
        return a[..., None, None] * state + kv_c, state

    _, states = lax.scan(step, jnp.zeros((B, H, dk, dv), jnp.float32), (a_chunk, kv))
    y = y + jnp.einsum('bhntd,bhnt,nbhde->bhnte', qc, jnp.exp(cum), states)
    return y.reshape(B, H, S, dv)


def mlstm_chunkwise(q, k, v, i_pre, log_f):
    B, H, S, d = q.shape
    n = S // CHUNK
    qc = (q * d ** -0.5).reshape(B, H, n, CHUNK, d)
    kc = k.reshape(B, H, n, CHUNK, d)
    vc = v.reshape(B, H, n, CHUNK, d)
    ic = i_pre.reshape(B, H, n, CHUNK)
    b = jnp.cumsum(log_f.reshape(B, H, n, CHUNK), axis=-1)
    causal = jnp.tril(jnp.ones((CHUNK, CHUNK), dtype=bool))
    logD = jnp.where(causal, b[..., :, None] - b[..., None, :] + ic[..., None, :], -jnp.inf)
    m_intra = jnp.max(logD, axis=-1)
    e = b[..., -1:] - b + ic
    g = jnp.max(e, axis=-1)
    w = jnp.exp(e - g[..., None])
    kv_loc = jnp.einsum('bhns,bhnsd,bhnse->nbhde', w, kc, vc)
    k_loc = jnp.einsum('bhns,bhnsd->nbhd', w, kc)
    b_end = b[..., -1].transpose(2, 0, 1)
    g_t = g.transpose(2, 0, 1)

    def step(carry, inp):
        C, nv, m = carry
        bl, gl, kvl, kl = inp
        m_new = jnp.maximum(bl + m, gl)
        a_old = jnp.exp(bl + m - m_new)
        a_loc = jnp.exp(gl - m_new)
        C_new = a_old[..., None, None] * C + a_loc[..., None, None] * kvl
        n_new = a_old[..., None] * nv + a_loc[..., None] * kl
        return (C_new, n_new, m_new), (C, nv, m)

    init = (jnp.zeros((B, H, d, d), jnp.float32), jnp.zeros((B, H, d), jnp.float32),
            jnp.zeros((B, H), jnp.float32))
    _, (C_in, n_in, m_in) = lax.scan(step, init, (b_end, g_t, kv_loc, k_loc))
    inter_log = b + m_in.transpose(1, 2, 0)[..., None]
    m_t = jnp.maximum(inter_log, m_intra)
    a_inter = jnp.exp(inter_log - m_t)
    P = jnp.exp(logD - m_t[..., None]) * jnp.einsum('bhntd,bhnsd->bhnts', qc, kc)
    num = (jnp.einsum('bhnts,bhnse->bhnte', P, vc)
           + a_inter[..., None] * jnp.einsum('bhntd,nbhde->bhnte', qc, C_in))
    den = P.sum(-1) + a_inter * jnp.einsum('bhntd,nbhd->bhnt', qc, n_in)
    h = num / jnp.maximum(jnp.abs(den), jnp.exp(-m_t))[..., None]
    return h.reshape(B, H, S, d)


def hybrid_mixer(u, w_in, w_out, fox_fb, mlstm_conv_w, mlstm_conv_b, mlstm_ib, mlstm_fb,
                 mlstm_norm_g, ret_norm_g, ssd_conv_w, ssd_conv_b, ssd_dt_bias, ssd_A_log,
                 ssd_D, ssd_norm_g):
    B, S, _ = u.shape
    W, H = GROUP_WIDTH, N_HEADS
    proj = (u @ w_in).astype(jnp.float32)
    fox_p, mlstm_p, ret_p, ssd_p = jnp.split(
        proj, [FOX_COLS, FOX_COLS + MLSTM_COLS, FOX_COLS + MLSTM_COLS + RET_COLS], axis=-1)

    fq, fk, fv, ff = jnp.split(fox_p, [W, 2 * W, 3 * W], axis=-1)
    fox_logf = jax.nn.log_sigmoid(ff + fox_fb).transpose(0, 2, 1)
    y_fox = merge_heads(forgetting_attention(to_heads(fq), to_heads(fk), to_heads(fv), fox_logf))

    mqk, mv, mi, mf, mo = jnp.split(mlstm_p, [2 * W, 3 * W, 3 * W + H, 3 * W + 2 * H], axis=-1)
    mqk = jax.nn.silu(causal_dwconv(mqk, mlstm_conv_w, mlstm_conv_b))
    mq, mk = jnp.split(mqk, 2, axis=-1)
    h_m = mlstm_chunkwise(to_heads(mq), to_heads(mk), to_heads(mv),
                          (mi + mlstm_ib).transpose(0, 2, 1),
                          jax.nn.log_sigmoid(mf + mlstm_fb).transpose(0, 2, 1))
    y_mlstm = jax.nn.sigmoid(mo) * merge_heads(head_rmsnorm(h_m, mlstm_norm_g))

    rq, rk, rv, rg = jnp.split(ret_p, 4, axis=-1)
    rq = rotary(to_heads(rq))
    rk = rotary(to_heads(rk)) * HEAD_DIM ** -0.5
    log_gamma = jnp.log(1.0 - 2.0 ** (-RET_DECAY_OFFSET - jnp.arange(H, dtype=jnp.float32)))
    ret_loga = jnp.broadcast_to(log_gamma[None, :, None], (B, H, S))
    h_r = chunked_decay_attention(rq, rk, to_heads(rv), ret_loga)
    y_ret = jax.nn.silu(rg) * merge_heads(head_rmsnorm(h_r, ret_norm_g))

    sz, sxbc, sdt = jnp.split(ssd_p, [W, 2 * W + 2 * SSD_GROUPS * SSD_STATE], axis=-1)
    sxbc = jax.nn.silu(causal_dwconv(sxbc, ssd_conv_w, ssd_conv_b))
    sx, sB, sC = jnp.split(sxbc, [W, W + SSD_GROUPS * SSD_STATE], axis=-1)
    dt = jax.nn.softplus(sdt + ssd_dt_bias).transpose(0, 2, 1)
    A = -jnp.exp(ssd_A_log.astype(jnp.float32))
    rep = H // SSD_GROUPS
    Bh = jnp.repeat(sB.reshape(B, S, SSD_GROUPS, SSD_STATE), rep, axis=2).transpose(0, 2, 1, 3)
    Ch = jnp.repeat(sC.reshape(B, S, SSD_GROUPS, SSD_STATE), rep, axis=2).transpose(0, 2, 1, 3)
    xh = to_heads(sx)
    h_s = chunked_decay_attention(Ch, Bh, xh * dt[..., None], dt * A[:, None])
    h_s = h_s + ssd_D[:, None, None] * xh
    y_ssd = rmsnorm(merge_heads(h_s) * jax.nn.silu(sz), ssd_norm_g)

    y = jnp.concatenate([y_fox, y_mlstm, y_ret, y_ssd], axis=-1)
    return y.astype(u.dtype) @ w_out


def setup_inputs(seed: int = 0) -> dict:
    key = jax.random.key(seed)
    ks = jax.random.split(key, 25)
    f32 = jnp.float32
    L, D, W, H = DEPTH, D_MODEL, GROUP_WIDTH, N_HEADS

    def nrm(k, shape, s):
        return s * jax.random.normal(k, shape, f32)

    dt0 = jnp.exp(jax.random.uniform(ks[20], (L, H), f32, math.log(1e-3), math.log(1e-1)))
    return {
        'x': nrm(ks[0], (BATCH, SEQ, D), 1.0),
        'c': nrm(ks[1], (BATCH, D), 1.0),
        'ada_w': nrm(ks[2], (L, D, N_MOD * D), 0.5 * D ** -0.5),
        'ada_b': nrm(ks[3], (L, N_MOD * D), 0.02),
        'norm_g': 1.0 + nrm(ks[4], (L, 3, D), 0.02),
        'ffn1_w13': nrm(ks[5], (L, D, 2 * D_FF), D ** -0.5),
        'ffn1_w2': nrm(ks[6], (L, D_FF, D), D_FF ** -0.5),
        'ffn2_w13': nrm(ks[7], (L, D, 2 * D_FF), D ** -0.5),
        'ffn2_w2': nrm(ks[8], (L, D_FF, D), D_FF ** -0.5),
        'w_in': nrm(ks[9], (L, D, D_IN_PROJ), D ** -0.5),
        'w_out': nrm(ks[10], (L, D_MIX, D), D_MIX ** -0.5),
        'fox_fb': 3.0 + nrm(ks[11], (L, H), 0.5),
        'mlstm_conv_w': nrm(ks[12], (L, CONV_K, 2 * W), CONV_K ** -0.5),
        'mlstm_conv_b': nrm(ks[13], (L, 2 * W), 0.02),
        'mlstm_ib': nrm(ks[14], (L, H), 0.1),
        'mlstm_fb': jnp.linspace(3.0, 6.0, H, dtype=f32)[None, :] + nrm(ks[15], (L, H), 0.1),
        'mlstm_norm_g': 1.0 + nrm(ks[16], (L, W), 0.02),
        'ret_norm_g': 1.0 + nrm(ks[17], (L, W), 0.02),
        'ssd_conv_w': nrm(ks[18], (L, CONV_K, W + 2 * SSD_GROUPS * SSD_STATE), CONV_K ** -0.5),
        'ssd_conv_b': nrm(ks[19], (L, W + 2 * SSD_GROUPS * SSD_STATE), 0.02),
        'ssd_dt_bias': dt0 + jnp.log(-jnp.expm1(-dt0)),
        'ssd_A_log': jnp.log(jax.random.uniform(ks[21], (L, H), f32, 1.0, 16.0)),
        'ssd_D': 1.0 + nrm(ks[22], (L, H), 0.1),
        'ssd_norm_g': 1.0 + nrm(ks[23], (L, W), 0.02),
        'final_g': 1.0 + nrm(ks[24], (D,), 0.02),
    }


def reference(x, c, ada_w, ada_b, norm_g, ffn1_w13, ffn1_w2, ffn2_w13, ffn2_w2, w_in, w_out,
              fox_fb, mlstm_conv_w, mlstm_conv_b, mlstm_ib, mlstm_fb, mlstm_norm_g, ret_norm_g,
              ssd_conv_w, ssd_conv_b, ssd_dt_bias, ssd_A_log, ssd_D, ssd_norm_g, final_g):
    c_act = jax.nn.silu(c)
    for l in range(DEPTH):
        cond = (c_act @ ada_w[l] + ada_b[l])[:, None, :]
        sh1, sc1, g1, sh2, sc2, g2, sh3, sc3, g3 = jnp.split(cond, N_MOD, axis=-1)
        h = modulate(rmsnorm(x, norm_g[l, 0]), sh1, sc1)
        x = x + 0.5 * g1 * swiglu(h, ffn1_w13[l], ffn1_w2[l])
        h = modulate(rmsnorm(x, norm_g[l, 1]), sh2, sc2)
        x = x + g2 * hybrid_mixer(h, w_in[l], w_out[l], fox_fb[l], mlstm_conv_w[l], mlstm_conv_b[l],
                                  mlstm_ib[l], mlstm_fb[l], mlstm_norm_g[l], ret_norm_g[l],
                                  ssd_conv_w[l], ssd_conv_b[l], ssd_dt_bias[l], ssd_A_log[l],
                                  ssd_D[l], ssd_norm_g[l])
        h = modulate(rmsnorm(x, norm_g[l, 2]), sh3, sc3)
        x = x + 0.5 * g3 * swiglu(h, ffn2_w13[l], ffn2_w2[l])
    return rmsnorm(x, final_g)
```

```python
import math
import os
import numpy as np
import ml_dtypes
from contextlib import ExitStack
import concourse.bass as bass
import concourse.mybir as mybir
from concourse.bass_utils import run_bass_kernel_spmd

F32 = mybir.dt.float32
BF16 = mybir.dt.bfloat16
AF = mybir.ActivationFunctionType
ALU = mybir.AluOpType

ENGS = ['pe', 'act', 'dve', 'pool', 'sp']
N_DMA_SEMS = 12
S = 2048
NTB = 4
EPS = 1e-6
NEG = -30000.0


class Fw:
    def __init__(self, nc, ctx, self_sync=True):
        self.nc = nc
        self.ctx = ctx
        self.self_sync = self_sync
        self.q = {e: [] for e in ENGS}
        self.cnt = {e: 0 for e in ENGS}
        self.semobj = {}
        for e in ['pe', 'act', 'dve', 'pool']:
            self.semobj['s_' + e] = ctx.enter_context(nc.semaphore('s_' + e))
        for p in 'dw':
            for i in range(N_DMA_SEMS):
                self.semobj['%s%d' % (p, i)] = ctx.enter_context(nc.semaphore('%s%d' % (p, i)))
        self.dcnt = {p: [0] * N_DMA_SEMS for p in 'dw'}
        self.dlast = {p: [None] * N_DMA_SEMS for p in 'dw'}
        self.dnext = {p: 0 for p in 'dw'}
        self.last_w = {}
        self.readers = {}
        self.seen = {e: {} for e in ENGS}
        self.n_wait = 0
        self.n_ins = 0

    def sb(self, name, shape, dt):
        return self.ctx.enter_context(self.nc.sbuf_tensor(name, shape, dt))

    def ps(self, name, shape, dt=F32):
        return self.ctx.enter_context(self.nc.psum_tensor(name, shape, dt))

    def _deps(self, e, reads, writes):
        toks = []
        for k in reads:
            t = self.last_w.get(k)
            if t is not None:
                toks.append(t)
            if isinstance(k, tuple) and k[0] == 'PS':
                toks.extend(self.readers.get(k, ()))
        for k in writes:
            t = self.last_w.get(k)
            if t is not None:
                toks.append(t)
            toks.extend(self.readers.get(k, ()))
        need = {}
        for (sk, val, te) in toks:
            if te == e and (e == 'pe' or not self.self_sync):
                continue
            if self.seen[e].get(sk, 0) >= val:
                continue
            if need.get(sk, 0) < val:
                need[sk] = val
        for sk, val in need.items():
            self.seen[e][sk] = val
        return list(need.items())

    def _commit(self, tok, reads, writes):
        for k in reads:
            lst = self.readers.setdefault(k, [])
            lst[:] = [t for t in lst if t[0] != tok[0]]
            lst.append(tok)
        for k in writes:
            self.last_w[k] = tok
            self.readers[k] = []

    def ops(self, e, fns, reads=(), writes=()):
        if not isinstance(fns, (list, tuple)):
            fns = [fns]
        waits = self._deps(e, reads, writes)
        self.cnt[e] += 1
        tok = ('s_' + e, self.cnt[e], e)
        n = len(fns)
        for i, fn in enumerate(fns):
            self.q[e].append((fn, waits if i == 0 else [], ('s_' + e, 1) if i == n - 1 else None))
        self._commit(tok, reads, writes)
        self.n_ins += n
        self.n_wait += len(waits)
        return tok

    op = ops

    def dma(self, e, fns, reads=(), writes=()):
        if not isinstance(fns, (list, tuple)):
            fns = [fns]
        p = 'w' if e == 'pool' else 'd'
        i = self.dnext[p]
        self.dnext[p] = (i + 1) % N_DMA_SEMS
        sk = '%s%d' % (p, i)
        waits = self._deps(e, reads, writes)
        prev = self.dlast[p][i]
        if prev is not None and self.seen[e].get(sk, 0) < prev[1]:
            waits.append((sk, prev[1]))
            self.seen[e][sk] = prev[1]
        self.dcnt[p][i] += 16 * len(fns)
        tok = (sk, self.dcnt[p][i], 'dma')
        self.dlast[p][i] = tok
        for j, fn in enumerate(fns):
            self.q[e].append((fn, waits if j == 0 else [], (sk, 16)))
        self._commit(tok, reads, writes)
        self.n_ins += len(fns)
        self.n_wait += len(waits)
        return tok

    def wait_all(self, e, toks):
        waits = []
        for (sk, val, te) in toks:
            if self.seen[e].get(sk, 0) < val:
                waits.append((sk, val))
                self.seen[e][sk] = val
        self.q[e].append((None, waits, None))

    def emit(self):
        nc = self.nc
        with nc.Block() as block:
            def run(e):
                def body(engine):
                    for (fn, waits, inc) in self.q[e]:
                        for (sk, val) in waits:
                            engine.wait_ge(self.semobj[sk], val)
                        if fn is None:
                            continue
                        ins = fn(engine)
                        if inc is not None:
                            ins.then_inc(self.semobj[inc[0]], inc[1])
                return body
            block.sync(run('sp'))
            block.scalar(run('act'))
            block.vector(run('dve'))
            block.gpsimd(run('pool'))
            block.tensor(run('pe'))


def _pp_layout():
    off = [0]

    def al(n):
        o = off[0]
        off[0] += n
        return o
    lay = {'c': al(8), 'fg': al(8), 'L': []}
    for l in range(4):
        lay['L'].append(dict(ada_b=al(72), ng=al(24), mcw=al(16), mcb=al(4), scw=al(16), scb=al(4),
                             mng=al(2), rng=al(2), sng=al(2), sD=al(2),
                             ffb=al(4), mib=al(4), mfb=al(4), sdtb=al(4), sAl=al(4)))
    lay['n'] = off[0]
    return lay


PPL = _pp_layout()
CB_ID, CB_MASK, CB_ONES, CB_SEL1, CB_SEL2, NCB = 0, 128, 128 + 960, 128 + 960 + 128, 128 + 960 + 256, 128 + 960 + 384
CF_TRI, CF_ONES, CF_RS, CF_COS, CF_SIN, NCF = 0, 128, 256, 320, 320 + 2048, 320 + 4096


def col(w, n):
    return np.ascontiguousarray(np.asarray(w, np.float32).reshape(n, 128).T)


def pack_pp(inp, b):
    pp = np.zeros((128, PPL['n']), np.float32)
    pp[:, PPL['c']:PPL['c'] + 8] = col(inp['c'][b], 8)
    pp[:, PPL['fg']:PPL['fg'] + 8] = col(inp['final_g'], 8)
    for l in range(4):
        o = PPL['L'][l]
        pp[:, o['ada_b']:o['ada_b'] + 72] = col(inp['ada_b'][l], 72)
        for i in range(3):
            pp[:, o['ng'] + 8 * i:o['ng'] + 8 * i + 8] = col(inp['norm_g'][l, i], 8)
        for j in range(4):
            pp[:, o['mcw'] + j:o['mcw'] + 16:4] = col(inp['mlstm_conv_w'][l, j], 4)
            pp[:, o['scw'] + j:o['scw'] + 16:4] = col(inp['ssd_conv_w'][l, j], 4)
        pp[:, o['mcb']:o['mcb'] + 4] = col(inp['mlstm_conv_b'][l], 4)
        pp[:, o['scb']:o['scb'] + 4] = col(inp['ssd_conv_b'][l], 4)
        pp[:, o['mng']:o['mng'] + 2] = col(inp['mlstm_norm_g'][l], 2)
        pp[:, o['rng']:o['rng'] + 2] = col(inp['ret_norm_g'][l], 2)
        pp[:, o['sng']:o['sng'] + 2] = col(inp['ssd_norm_g'][l], 2)
        pp[:, o['sD']:o['sD'] + 2] = col(np.repeat(inp['ssd_D'][l], 64), 2)
        for nm, key in (('ffb', 'fox_fb'), ('mib', 'mlstm_ib'), ('mfb', 'mlstm_fb'), ('sdtb', 'ssd_dt_bias'),
                        ('sAl', 'ssd_A_log')):
            pp[:, o[nm]:o[nm] + 4] = np.broadcast_to(inp[key][l][None, :], (128, 4))
    return pp


def split3(x):
    x = np.asarray(x, np.float32)
    hi = x.astype(ml_dtypes.bfloat16)
    r = x - hi.astype(np.float32)
    mid = r.astype(ml_dtypes.bfloat16)
    r2 = r - mid.astype(np.float32)
    lo = r2.astype(ml_dtypes.bfloat16)
    return hi, mid, lo


def make_consts():
    cb = np.zeros((128, NCB), np.float32)
    cb[:, CB_ID:CB_ID + 128] = np.eye(128)
    kk = (np.arange(128) % 64)[:, None]
    uu = np.arange(960)[None, :]
    cb[:, CB_MASK:CB_MASK + 960] = np.where(kk <= uu - 448, 0.0, NEG)
    for k in range(128):
        cb[k, CB_SEL1 + (k % 64)] = 1.0
        cb[k, CB_SEL2 + (k % 64) + 64] = 1.0
    cb[:, CB_ONES:CB_ONES + 128] = 1.0
    cb = cb.astype(ml_dtypes.bfloat16)
    cf = np.zeros((128, NCF), np.float32)
    cf[:, CF_TRI:CF_TRI + 128] = (np.arange(128)[:, None] <= np.arange(128)[None, :]).astype(np.float32)
    cf[:, CF_ONES:CF_ONES + 128] = 1.0
    d = 64
    inv = (1.0 / (np.float32(10000.0) ** (np.arange(0, d, 2, dtype=np.float32) / np.float32(d)))).astype(np.float32)
    ang = np.arange(S, dtype=np.float32)[:, None] * inv[None, :]
    cos, sin = np.cos(ang).astype(np.float32), np.sin(ang).astype(np.float32)
    for p in range(128):
        i = p % 32
        cf[p, CF_COS:CF_COS + S] = cos[:, i]
        cf[p, CF_SIN:CF_SIN + S] = (-sin[:, i]) if (p % 64) < 32 else sin[:, i]
    ra = np.zeros((128, 2, S), ml_dtypes.bfloat16)
    t = np.arange(S, dtype=np.float64)
    rs = np.zeros((128, 16, 4), np.float32)
    for h in range(4):
        lg = np.log(np.float32(1.0) - np.float32(2.0) ** np.float32(-5.0 - h)).astype(np.float32)
        T = (t * np.float64(lg)).astype(np.float32)
        Sv = (-(t * np.float64(lg)) + math.log(0.125)).astype(np.float32)
        for r, v in enumerate(split3(T)):
            ra[64 * (h % 2) + r, h // 2] = v
        rs[:, :, h] = Sv.reshape(16, 128).T
    cf[:, CF_RS:CF_RS + 64] = rs.reshape(128, 64)
    return cb, cf, ra


def mm(out, lhsT, rhs, start, stop, **kw):
    return lambda t: t.matmul(out, lhsT=lhsT, rhs=rhs, start=start, stop=stop, **kw)


def build(L=4, stop=None, self_sync=True, NLW=4):
    nc = bass.Bass("TRN2", target_bir_lowering=False)

    def D(name, shape, dt, kind="ExternalInput"):
        return nc.dram_tensor(name, shape, dt, kind=kind).ap()
    xT = D("xT", [1024, S], F32)
    ppd = D("pp", [128, PPL['n']], F32)
    cbd = D("cb", [128, NCB], BF16)
    cfd = D("cf", [128, NCF], F32)
    rad = D("retaug", [128, 2, S], BF16)
    ada_w = D("ada_w", [NLW, 1024, 9216], F32)
    w13d = [D("ffn1_w13", [NLW, 1024, 5632], F32), D("ffn2_w13", [NLW, 1024, 5632], F32)]
    w2d = [D("ffn1_w2", [NLW, 2816, 1024], F32), D("ffn2_w2", [NLW, 2816, 1024], F32)]
    w_in = D("w_in", [NLW, 1024, 3600], F32)
    w_out = D("w_out", [NLW, 1024, 1024], F32)
    yT = D("yT", [1024, S], F32, kind="ExternalOutput")

    with ExitStack() as ctx:
        fw = Fw(nc, ctx, self_sync=self_sync)
        X = fw.sb("X", [128, 8, S], F32)
        H = fw.sb("H", [128, 8, S], BF16)
        W8 = [fw.sb("W8_%d" % i, [128, 8, 512], BF16) for i in range(2)]
        W2 = [fw.sb("W2_%d" % i, [128, 2, 1024], BF16) for i in range(2)]
        A = [fw.sb("A_%d" % i, [128, 2, S], BF16) for i in range(4)]
        AUG = fw.sb("AUG", [128, 2, S], BF16)
        V = fw.sb("V", [128, 16, 384], BF16)
        SPL = fw.sb("SPL", [128, 16, 128], BF16)
        NT = 5
        T = [fw.sb("T_%d" % i, [128, 512], F32) for i in range(NT)]
        NPT = 4
        PTB = [fw.sb("PT_%d" % i, [128, 512], BF16) for i in range(NPT)]
        CS = fw.sb("CS", [128, 516], F32)
        RSTD = CS[:, 0:512]
        ROPE = fw.sb("ROPE", [128, 2, 512], F32)
        PP = fw.sb("PP", [128, PPL['n']], F32)
        CB = fw.sb("CB", [128, NCB], BF16)
        CF = fw.sb("CF", [128, 320], F32)
        COND = fw.sb("COND", [128, 4, 72], F32)
        CA2 = fw.sb("CA2", [128, 8, 2], F32)
        AB = fw.sb("AB", [128, 8], F32)
        GT8 = fw.sb("GT8", [128, 8], F32)
        GPT = fw.sb("GPT", [128, 16, 8], F32)
        G = [fw.sb("G_%d" % i, [128, 16, 4], F32) for i in range(7)]
        PRE = fw.sb("PRE", [128, 16, 4], F32)
        NPS = 7
        PS = [fw.ps("PS_%d" % i, [128, 512], F32) for i in range(NPS)]
        PSB = fw.ps("PSB", [128, 1024], BF16)
        PS.append(PSB[:].bitcast(F32))
        ROTB = [0, 1, 2, 3, 4, 7]
        IDENT = CB[:, CB_ID:CB_ID + 128]
        ONESB = CB[:, CB_ONES:CB_ONES + 128]
        TRI = CF[:, CF_TRI:CF_TRI + 128]
        ONESF = CF[:, CF_ONES:CF_ONES + 128]

        rot = {'ps': 0, 't': 0, 'pt': 0, 'w8': 0, 'w2': 0}

        def nxt(k, n):
            v = rot[k]
            rot[k] = (v + 1) % n
            return v

        rot['po'] = 0

        def psn():
            return ROTB[nxt('ps', len(ROTB))]

        def pon():
            return NPS - 2 + nxt('po', 2)

        def tmpf():
            return nxt('t', NT)

        def ptn():
            return nxt('pt', NPT)

        def tbs(tb):
            return slice(tb * 512, (tb + 1) * 512)

        fw.dma('sp', lambda e: e.dma_start(out=PP[:], in_=ppd), writes=['PP'])
        fw.dma('sp', lambda e: e.dma_start(out=CB[:], in_=cbd), writes=['CB'])
        fw.dma('sp', lambda e: e.dma_start(out=CF[:], in_=cfd[:, 0:320]), writes=['CF'])
        for c in range(8):
            fw.dma('act', lambda e, c=c: e.dma_start(out=X[:, c, :], in_=xT[c * 128:(c + 1) * 128, :]),
                   writes=[('X', c, tb) for tb in range(4)])
        fw.op('pool', lambda g: g.memset(V[:, :, 64:128], 1.0), writes=[('V', i) for i in range(16)])
        fw.op('pool', lambda g: g.memset(V[:, :, 256:320], 1.0), writes=[('V', i) for i in range(16)])
        print("sbuf bytes remaining", nc.sbuf_bytes_remaining, flush=True)
        fw.op('pool', lambda g: g.memset(SPL[:], 0.0), writes=[('SPL', i) for i in range(16)])
        fw.op('pool', lambda g: g.memset(CS[:], 0.0), writes=['CS'])
        for j in range(2):
            fw.op('act', lambda a, j=j: a.activation(out=CA2[:, :, j], in_=PP[:, PPL['c']:PPL['c'] + 8], func=AF.Silu),
                  reads=['PP'], writes=['CA2'])
        Hf = H[:].bitcast(F32)
        CPSI = NPS - 1
        gi = 0
        for l in range(1):
            awl = ada_w[l].rearrange("(k p) n -> p k n", p=128)
            for g in range(18):
                b = gi % 2
                gi += 1
                keys = [('H', k, 2 * b + u) for k in range(8) for u in range(2)]
                fw.dma('sp', lambda e, b=b, g=g, awl=awl: e.dma_start(out=Hf[:, :, b * 512:(b + 1) * 512],
                                                                     in_=awl[:, :, g * 512:(g + 1) * 512]),
                       writes=keys)
                fns = []
                for j in range(4):
                    n = g * 4 + j
                    for k in range(8):
                        fns.append(mm(PS[CPSI][:, 2 * n:2 * n + 2],
                                      lhsT=Hf[:, k, b * 512 + j * 128:b * 512 + (j + 1) * 128],
                                      rhs=CA2[:, k, :], start=(k == 0), stop=(k == 7)))
                fw.ops('pe', fns, reads=keys + ['CA2'], writes=[('PS', CPSI)])
            ab = PPL['L'][l]['ada_b']
            fw.op('dve', lambda v, l=l, ab=ab: v.tensor_tensor(out=COND[:, l, :], in0=PS[CPSI][:, 0:144:2],
                                                             in1=PP[:, ab:ab + 72], op=ALU.add),
                  reads=[('PS', CPSI), 'PP'], writes=[('COND', l)])

        def cond_piece(l, p):
            bi = 2 + p % 2
            Af8 = A[bi][:].bitcast(F32).rearrange("p a (k n) -> p (a k) n", n=256)
            keys = [('A', bi, c, t) for c in range(2) for t in range(4)]
            awl = ada_w[l].rearrange("(k p) n -> p k n", p=128)
            fw.dma('sp', lambda e: e.dma_start(out=Af8, in_=awl[:, :, p * 256:(p + 1) * 256]), writes=keys)
            fns = []
            for j in range(2):
                n = 2 * p + j
                for k in range(8):
                    fns.append(mm(PS[CPSI][:, 2 * n:2 * n + 2], lhsT=Af8[:, k, j * 128:(j + 1) * 128], rhs=CA2[:, k, :],
                                  start=(k == 0), stop=(k == 7)))
            fw.ops('pe', fns, reads=keys + ['CA2'], writes=[('PS', CPSI)])

        def cond_finish(l):
            ab = PPL['L'][l]['ada_b']
            fw.op('dve', lambda v: v.tensor_tensor(out=COND[:, l, :], in0=PS[CPSI][:, 0:144:2],
                                                  in1=PP[:, ab:ab + 72], op=ALU.add),
                  reads=[('PS', CPSI), 'PP'], writes=[('COND', l)])

        def norm_prep(l, i):
            o = PPL['L'][l]
            sc0 = (3 * i + 1) * 8
            fw.op('dve', lambda v: v.scalar_tensor_tensor(out=AB[:], in0=COND[:, l, sc0:sc0 + 8], scalar=1.0,
                                                         in1=PP[:, o['ng'] + 8 * i:o['ng'] + 8 * i + 8],
                                                         op0=ALU.add, op1=ALU.mult),
                  reads=[('COND', l), 'PP'], writes=['AB'])

        def norm_tb(l, i, tb):
            sh0 = (3 * i) * 8
            SQv = A[3][:].rearrange("p a (b n) -> p (a b) n", n=512)
            sqkeys = [('A', 3, c, t) for c in range(2) for t in range(4)]
            fw.op('act', lambda a: a.activation(out=SQv, in_=X[:, :, tbs(tb)], func=AF.Square),
                  reads=[('X', c, tb) for c in range(8)], writes=sqkeys)
            ps = psn()
            fw.ops('pe', [mm(PS[ps][:, :], lhsT=ONESB, rhs=SQv[:, c, :], start=(c == 0), stop=(c == 7))
                          for c in range(8)], reads=sqkeys + ['CB'], writes=[('PS', ps)])
            fw.op('act', lambda a: a.activation(out=RSTD, in_=PS[ps][:], func=AF.Sqrt,
                                                scale=1.0 / 1024.0, bias=EPSB[:, 0:1]),
                  reads=[('PS', ps), 'EPSB'], writes=['CS'])
            fw.op('dve', lambda v: v.reciprocal(out=RSTD, in_=RSTD), reads=['CS'], writes=['CS'])
            for c in range(8):
                t2 = tmpf()
                fw.op('dve', lambda v, c=c, t2=t2: v.tensor_tensor(out=T[t2][:], in0=X[:, c, tbs(tb)],
                                                                   in1=RSTD, op=ALU.mult),
                      reads=[('X', c, tb), 'CS'], writes=[('T', t2)])
                fw.op('act', lambda a, c=c, t2=t2: a.activation(out=H[:, c, tbs(tb)], in_=T[t2][:],
                                                                func=AF.Identity, scale=AB[:, c:c + 1],
                                                                bias=COND[:, l, sh0 + c:sh0 + c + 1]),
                      reads=[('T', t2), 'AB', ('COND', l)], writes=[('H', c, tb)])

        def norm_mod(l, i):
            norm_prep(l, i)
            for tb in range(4):
                norm_tb(l, i, tb)

        EPSB = fw.sb("EPSB", [128, 1], F32)
        fw.op('pool', lambda g: g.memset(EPSB[:], EPS), writes=['EPSB'])

        def emit_w2(b, ai, tb):
            for m in range(8):
                ps = psn()
                fw.ops('pe', [mm(PS[ps][:, :], lhsT=W2[b][:, c, m * 128:(m + 1) * 128], rhs=A[ai][:, c, tbs(tb)],
                                 start=(c == 0), stop=(c == 1)) for c in range(2)],
                       reads=[('W2', b), ('A', ai, 0, tb), ('A', ai, 1, tb)], writes=[('PS', ps)])
                fw.op('dve', lambda v, ps=ps, m=m, tb=tb: v.scalar_tensor_tensor(
                    out=X[:, m, tbs(tb)], in0=PS[ps][:], scalar=GT8[:, m:m + 1], in1=X[:, m, tbs(tb)],
                    op0=ALU.mult, op1=ALU.add),
                    reads=[('PS', ps), 'GT8', ('X', m, tb)], writes=[('X', m, tb)])

        def set_gate(l, part, scale):
            fw.op('dve', lambda v: v.tensor_scalar(out=GT8[:], in0=COND[:, l, part * 8:part * 8 + 8], scalar1=scale,
                                                  scalar2=None, op0=ALU.mult),
                  reads=[('COND', l)], writes=['GT8'])

        def ffn(l, f, pre=None):
            Wa = w13d[f][l].rearrange("(k p) n -> p k n", p=128)
            Wb = w2d[f][l].rearrange("(k p) n -> p k n", p=128)
            set_gate(l, 2 if f == 0 else 8, 0.5)
            prev = None
            for j in range(int(os.environ.get('FFN_NJ', '11'))):
                b = nxt('w8', 2)
                fw.dma('pool', [lambda e, b=b, j=j: e.dma_start(out=W8[b][:, :, 0:256], in_=Wa[:, :, j * 256:(j + 1) * 256]),
                                lambda e, b=b, j=j: e.dma_start(out=W8[b][:, :, 256:512],
                                                                in_=Wa[:, :, 2816 + j * 256:2816 + (j + 1) * 256])],
                       writes=[('W8', b)])
                b2 = nxt('w2', 2)
                fw.dma('pool', lambda e, b2=b2, j=j: e.dma_start(out=W2[b2][:], in_=Wb[:, 2 * j:2 * j + 2, :]),
                       writes=[('W2', b2)])
                ai = j % 2
                for tb in range(4):
                    for fn_ in (pre or {}).get(4 * j + tb, ()):
                        fn_()
                    if f == 1 and l + 1 < L and 4 * j + tb < 36:
                        cond_piece(l + 1, 4 * j + tb)
                    pss = []
                    for q in range(4):
                        ps = psn()
                        pss.append(ps)
                        fw.ops('pe', [mm(PS[ps][:, :], lhsT=W8[b][:, k, q * 128:(q + 1) * 128], rhs=H[:, k, tbs(tb)],
                                         start=(k == 0), stop=(k == 7)) for k in range(8)],
                               reads=[('W8', b)] + [('H', k, tb) for k in range(8)], writes=[('PS', ps)])
                    for c in range(2):
                        t = tmpf()
                        fw.op('act', lambda a, t=t, p=pss[2 + c]: a.activation(out=T[t][:], in_=PS[p][:], func=AF.Silu),
                              reads=[('PS', pss[2 + c])], writes=[('T', t)])
                        fw.op('dve', lambda v, t=t, p=pss[c], c=c, tb=tb, ai=ai: v.tensor_tensor(
                            out=A[ai][:, c, tbs(tb)], in0=PS[p][:], in1=T[t][:], op=ALU.mult),
                            reads=[('PS', pss[c]), ('T', t)], writes=[('A', ai, c, tb)])
                    if prev is not None:
                        emit_w2(*prev)
                    prev = (b2, ai, tb)
            emit_w2(*prev)
            if f == 1 and l + 1 < L:
                cond_finish(l + 1)

        def load_w8(Win, pieces):
            b = nxt('w8', 2)
            fw.dma('pool', [lambda e, b=b, d=d, s=s, n=n: e.dma_start(out=W8[b][:, :, d:d + n], in_=Win[:, :, s:s + n])
                            for (d, s, n) in pieces], writes=[('W8', b)])
            return b

        def proj_chunk(b, wcol, tb):
            ps = psn()
            fw.ops('pe', [mm(PS[ps][:, :], lhsT=W8[b][:, k, wcol:wcol + 128], rhs=H[:, k, tbs(tb)],
                             start=(k == 0), stop=(k == 7)) for k in range(8)],
                   reads=[('W8', b)] + [('H', k, tb) for k in range(8)], writes=[('PS', ps)])
            return ps

        def proj_tm(Win, vcol, gcol, ng, pre=None):
            pieces = []
            nv = 0
            if vcol is not None:
                pieces.append((0, vcol, 256))
                nv = 256
            if ng:
                pieces.append((nv, gcol, ng))
            b = load_w8(Win, pieces)
            n = nv + ng
            for i in range(16):
                for fn_ in (pre or {}).get(i, ()):
                    fn_()
                ps = psn()
                fw.ops('pe', [mm(PS[ps][:, 0:n], lhsT=H[:, k, i * 128:(i + 1) * 128], rhs=W8[b][:, k, 0:n],
                                 start=(k == 0), stop=(k == 7)) for k in range(8)],
                       reads=[('W8', b)] + [('H', k, i // 4) for k in range(8)], writes=[('PS', ps)])
                if nv:
                    for u in range(2):
                        fw.op('act', lambda a, ps=ps, i=i, u=u: a.activation(
                            out=v_pair(i, u),
                            in_=PS[ps][:, 128 * u:128 * u + 128].rearrange("p (a b) -> p a b", b=64), func=AF.Copy),
                            reads=[('PS', ps)], writes=[('V', i)])
                if ng:
                    fw.op('act', lambda a, ps=ps, i=i: a.activation(out=GPT[:, i, 0:ng], in_=PS[ps][:, nv:nv + ng], func=AF.Copy),
                          reads=[('PS', ps)], writes=['GPT'])

        def bcast4(c0):
            return PP[:, c0:c0 + 4].unsqueeze(1).to_broadcast([128, 16, 4])

        def softplus_parts(z, tmp_a, out_l):
            fw.op('act', lambda a: a.activation(out=G[tmp_a][:], in_=G[z][:], func=AF.Abs),
                  reads=[('G', z)], writes=[('G', tmp_a)])
            fw.op('act', lambda a: a.activation(out=G[tmp_a][:], in_=G[tmp_a][:], func=AF.Exp, scale=-1.0),
                  reads=[('G', tmp_a)], writes=[('G', tmp_a)])
            fw.op('act', lambda a: a.activation(out=G[out_l][:], in_=G[tmp_a][:], func=AF.Ln, bias=ONE1[:, 0:1], scale=1.0),
                  reads=[('G', tmp_a), 'ONE1'], writes=[('G', out_l)])

        ONE1 = fw.sb("ONE1", [128, 1], F32)
        fw.op('pool', lambda g: g.memset(ONE1[:], 1.0), writes=['ONE1'])

        def cumsum(src, dst):
            p1 = psn()
            p2 = psn()
            flat = G[src][:].rearrange("p a b -> p (a b)")
            fw.ops('pe', [mm(PS[p1][:, 0:64], lhsT=TRI, rhs=flat, start=True, stop=True)],
                   reads=[('G', src), 'CF'], writes=[('PS', p1)])
            fw.ops('pe', [mm(PS[p2][:, 0:64], lhsT=ONESF, rhs=flat, start=True, stop=True)],
                   reads=[('G', src), 'CF'], writes=[('PS', p2)])
            tot = PS[p2][:, 0:64].rearrange("p (a b) -> p a b", b=4)
            fw.op('dve', lambda v: v.memset(PRE[:, 0, :], 0.0), writes=['PRE'])
            for i in range(1, 16):
                fw.op('dve', lambda v, i=i: v.tensor_tensor(out=PRE[:, i, :], in0=PRE[:, i - 1, :], in1=tot[:, i - 1, :],
                                                           op=ALU.add),
                      reads=['PRE', ('PS', p2)], writes=['PRE'])
            fw.op('dve', lambda v: v.tensor_tensor(out=G[dst][:], in0=PS[p1][:, 0:64].rearrange("p (a b) -> p a b", b=4),
                                                  in1=PRE[:], op=ALU.add),
                  reads=['PRE', ('PS', p1)], writes=[('G', dst)])

        SB3 = [fw.sb("SB3_%d" % i, [128, 16, 4], BF16) for i in range(3)]

        def build_aug(val, t1, t2):
            spl_keys = [('SPL', i) for i in range(16)]
            fw.op('dve', lambda v: v.tensor_copy(out=SB3[0][:], in_=G[val][:]), reads=[('G', val)], writes=[('SB3', 0)])
            fw.op('dve', lambda v: v.tensor_tensor(out=G[t1][:], in0=G[val][:], in1=SB3[0][:], op=ALU.subtract),
                  reads=[('G', val), ('SB3', 0)], writes=[('G', t1)])
            fw.op('dve', lambda v: v.tensor_copy(out=SB3[1][:], in_=G[t1][:]), reads=[('G', t1)], writes=[('SB3', 1)])
            fw.op('dve', lambda v: v.tensor_tensor(out=G[t2][:], in0=G[t1][:], in1=SB3[1][:], op=ALU.subtract),
                  reads=[('G', t1), ('SB3', 1)], writes=[('G', t2)])
            fw.op('dve', lambda v: v.tensor_copy(out=SB3[2][:], in_=G[t2][:]), reads=[('G', t2)], writes=[('SB3', 2)])
            v4 = SPL[:].rearrange("p a (h r) -> p a h r", r=64)
            for j in range(2):
                for r in range(3):
                    fw.op('dve', lambda v, j=j, r=r: v.tensor_copy(out=v4[:, :, :, r], in_=SB3[r][:, :, 2 * j:2 * j + 2]),
                          reads=[('SB3', r)], writes=spl_keys)
                for tb in range(4):
                    half = tb % 2
                    fw.ops('pe', [lambda t, i=i, half=half: t.transpose(
                        out=PSB[:, half * 512 + (i % 4) * 128:half * 512 + (i % 4 + 1) * 128], in_=SPL[:, i, :], identity=IDENT)
                        for i in range(4 * tb, 4 * tb + 4)],
                        reads=spl_keys + ['CB'], writes=[('PS', 7)])
                    fw.op('act', lambda a, tb=tb, half=half, j=j: a.activation(out=AUG[:, j, tbs(tb)],
                                                                               in_=PSB[:, half * 512:(half + 1) * 512],
                                                                               func=AF.Copy),
                          reads=[('PS', 7)], writes=[('AUG', j, tb)])

        def attention(mode, qk, vfn, M, post, pre_tb=None, share_qk=False):
            LOOK = 2
            if share_qk:
                tasks = [(tb, 2 * g + u, sc) for tb in range(4) for g in range(2) for sc in range(4 * tb + 4)
                         for u in range(2)]
            else:
                tasks = [(tb, h, sc) for tb in range(4) for h in range(4) for sc in range(4 * tb + 4)]
            acc = {}
            shared = {}

            def phase_a(tb, h, sc):
                qa, ka = qk(h)
                pq = 64 * (h % 2)
                slab = h // 2
                d = sc - 4 * tb
                scs = slice(sc * 128, (sc + 1) * 128)
                trow = lambda ps, first, last: mm(PS[ps][:, :], lhsT=CB[pq:pq + 64, CB_ONES:CB_ONES + 128],
                                                  rhs=AUG[pq:pq + 64, slab, tbs(tb)], start=first, stop=last)

                def maskmms(ps):
                    c1 = CB_MASK + 448 - 128 * d
                    c2 = c1 - 64
                    return [mm(PS[ps][:, :], lhsT=CB[pq:pq + 64, CB_SEL1:CB_SEL1 + 128], rhs=CB[pq:pq + 64, c1:c1 + 512],
                               start=False, stop=False),
                            mm(PS[ps][:, :], lhsT=CB[pq:pq + 64, CB_SEL2:CB_SEL2 + 128], rhs=CB[pq:pq + 64, c2:c2 + 512],
                               start=False, stop=True)]
                rd_qk = [qk_key(h, 'q', tb), qk_key(h, 'k', sc // 4)]
                rd_aug = [('AUG', slab, tb)]
                sbias = G[5][:, sc, h:h + 1]
                pt = ptn()
                if mode == 'softmax':
                    ps = psn()
                    fns = [mm(PS[ps][:, :], lhsT=ka[:, scs], rhs=qa[:, tbs(tb)], start=True, stop=False),
                           trow(ps, False, d < 0)]
                    if d >= 0:
                        fns += maskmms(ps)
                    fw.ops('pe', fns, reads=rd_qk + rd_aug + ['CB'], writes=[('PS', ps)])
                    fw.op('act', lambda a, ps=ps, pt=pt: a.activation(out=PTB[pt][:], in_=PS[ps][:], func=AF.Exp,
                                                                      bias=sbias, scale=1.0),
                          reads=[('PS', ps), ('G', 5)], writes=[('PT', pt)])
                else:
                    if share_qk and h % 2 == 1:
                        ps = shared['ps']
                    else:
                        ps = psn()
                        shared['ps'] = ps
                        fw.ops('pe', [mm(PS[ps][:, :], lhsT=ka[:, scs], rhs=qa[:, tbs(tb)], start=True, stop=True)],
                               reads=rd_qk, writes=[('PS', ps)])
                    pl = psn()
                    fns = [trow(pl, True, d < 0)]
                    if d >= 0:
                        fns += maskmms(pl)
                    fw.ops('pe', fns, reads=rd_aug + ['CB'], writes=[('PS', pl)])
                    e = tmpf()
                    fw.op('act', lambda a, pl=pl, e=e: a.activation(out=T[e][:], in_=PS[pl][:], func=AF.Exp,
                                                                    bias=sbias, scale=1.0),
                          reads=[('PS', pl), ('G', 5)], writes=[('T', e)])
                    fw.op('dve', lambda v, ps=ps, e=e, pt=pt: v.tensor_tensor(out=PTB[pt][:], in0=PS[ps][:],
                                                                            in1=T[e][:], op=ALU.mult),
                          reads=[('PS', ps), ('T', e)], writes=[('PT', pt)])
                return pt

            def phase_b(tb, h, sc, pt):
                nsc = 4 * tb + 4
                if sc == 0:
                    acc[h] = pon()
                po = acc[h]
                fw.ops('pe', [mm(PS[po][:, :], lhsT=vfn(h, sc), rhs=PTB[pt][:], start=(sc == 0), stop=(sc == nsc - 1))],
                       reads=[('PT', pt), ('V', sc)], writes=[('PS', po)])
                if sc == nsc - 1:
                    cont = post(h, tb, po)
                    if cont is not None:
                        deferred.append(cont)

            pend = []
            deferred = []
            for i0 in range(0, len(tasks), LOOK):
                for (tb, h, sc) in tasks[i0:i0 + LOOK]:
                    pend.append((tb, h, sc, phase_a(tb, h, sc)))
                while deferred:
                    deferred.pop(0)()
                while len(pend) > LOOK:
                    phase_b(*pend.pop(0))
            while pend:
                phase_b(*pend.pop(0))
                while deferred:
                    deferred.pop(0)()

        def qk_key(h, which, blk):
            return ('QK', h, which, blk)

        def head_norm_gate(l, h, tb, tsrc, gcol):
            pb = 64 * (h % 2)
            c = h // 2
            t2 = tmpf()
            fw.op('act', lambda a: a.activation(out=T[t2][pb:pb + 64, :], in_=T[tsrc][pb:pb + 64, :], func=AF.Square),
                  reads=[('T', tsrc)], writes=[('T', t2)])
            def cont():
                head_norm_gate2(l, h, tb, tsrc, gcol, t2)
            return cont

        def head_norm_gate2(l, h, tb, tsrc, gcol, t2):
            pb = 64 * (h % 2)
            c = h // 2
            pn = psn()
            fw.ops('pe', [mm(PS[pn][0:64, :], lhsT=CF[pb:pb + 64, CF_ONES:CF_ONES + 64], rhs=T[t2][pb:pb + 64, :],
                             start=True, stop=True)], reads=[('T', t2), 'CF'], writes=[('PS', pn)])
            fw.op('act', lambda a: a.activation(out=T[t2][pb:pb + 64, :], in_=PS[pn][0:64, :], func=AF.Sqrt,
                                                scale=1.0 / 64.0, bias=EPSB[pb:pb + 64, 0:1]),
                  reads=[('PS', pn), 'EPSB'], writes=[('T', t2)])
            fw.op('dve', lambda v: v.reciprocal(out=T[t2][pb:pb + 64, :], in_=T[t2][pb:pb + 64, :]),
                  reads=[('T', t2)], writes=[('T', t2)])
            fw.op('dve', lambda v: v.scalar_tensor_tensor(out=T[tsrc][pb:pb + 64, :], in0=T[tsrc][pb:pb + 64, :],
                                                         scalar=PP[pb:pb + 64, gcol + c:gcol + c + 1],
                                                         in1=T[t2][pb:pb + 64, :], op0=ALU.mult, op1=ALU.mult),
                  reads=[('T', tsrc), ('T', t2), 'PP'], writes=[('T', tsrc)])
            fw.op('dve', lambda v: v.tensor_tensor(out=A[2][pb:pb + 64, c, tbs(tb)], in0=T[tsrc][pb:pb + 64, :],
                                                  in1=A[3][pb:pb + 64, c, tbs(tb)], op=ALU.mult),
                  reads=[('T', tsrc), ('A', 3, c, tb)], writes=[('A', 2, c, tb)])

        def conv_silu(l, ps, wcol, bcol, cc, tb, out_ap, wkeys):
            if tb == 0:
                fw.op('dve', lambda v: v.memset(CS[:, 0:3], 0.0), writes=['CS'])
            fw.op('act', lambda a: a.activation(out=CS[:, 3:515], in_=PS[ps][:], func=AF.Copy),
                  reads=[('PS', ps)], writes=['CS'])
            t = tmpf()
            fw.op('dve', lambda v: v.tensor_scalar(out=T[t][:], in0=CS[:, 0:512], scalar1=PP[:, wcol + cc * 4:wcol + cc * 4 + 1],
                                                  scalar2=None, op0=ALU.mult), reads=['CS', 'PP'], writes=[('T', t)])
            for j in range(1, 4):
                fw.op('dve', lambda v, j=j: v.scalar_tensor_tensor(out=T[t][:], in0=CS[:, j:j + 512],
                                                                  scalar=PP[:, wcol + cc * 4 + j:wcol + cc * 4 + j + 1],
                                                                  in1=T[t][:], op0=ALU.mult, op1=ALU.add),
                      reads=['CS', 'PP', ('T', t)], writes=[('T', t)])
            if tb < 3:
                fw.op('dve', lambda v: v.tensor_copy(out=CS[:, 0:3], in_=CS[:, 512:515]), reads=['CS'], writes=['CS'])
            fw.op('act', lambda a: a.activation(out=out_ap, in_=T[t][:], func=AF.Silu, bias=PP[:, bcol + cc:bcol + cc + 1],
                                                scale=1.0), reads=[('T', t), 'PP'], writes=wkeys)

        def out_proj(l, mi):
            Wout = w_out[l].rearrange("(k p) n -> p k n", p=128)
            b2 = nxt('w2', 2)
            fw.dma('pool', lambda e: e.dma_start(out=W2[b2][:], in_=Wout[:, 2 * mi:2 * mi + 2, :]), writes=[('W2', b2)])
            for tb in range(4):
                emit_w2(b2, 2, tb)

        def std_qk(h):
            return A[0][64 * (h % 2):64 * (h % 2) + 64, h // 2, :], A[1][64 * (h % 2):64 * (h % 2) + 64, h // 2, :]

        def qk_writes(which, c, tb):
            return [qk_key(2 * c, which, tb), qk_key(2 * c + 1, which, tb), ('A', 0 if which == 'q' else 1, c, tb)]

        def v_pair(i, u):
            base = V[:, i, 192 * u:192 * u + 64]
            return bass.AP(tensor=base.tensor, offset=base.offset, ap=[list(base.ap[0]), [128, 2], [1, 64]])

        VAUG0 = [0, 64, 192, 256]
        VCOL = [0, 128, 192, 320]

        def v_aug(h, sc):
            return V[:, sc, VAUG0[h]:VAUG0[h] + 128]

        def v_plain(h, sc):
            return V[:, sc, VCOL[h]:VCOL[h] + 64]

        def mixer(l):
            o = PPL['L'][l]
            Win = w_in[l].rearrange("(k p) n -> p k n", p=128)
            set_gate(l, 5, 1.0)
            proj_tm(Win, 512, 768, 4, pre={0: [lambda: norm_tb(l, 1, 0), lambda: norm_tb(l, 1, 1)],
                                            4: [lambda: norm_tb(l, 1, 2)], 8: [lambda: norm_tb(l, 1, 3)]})
            if stop == 'fox_g1':
                return
            fw.op('dve', lambda v: v.tensor_tensor(out=G[0][:], in0=GPT[:, :, 0:4], in1=bcast4(o['ffb']), op=ALU.add),
                  reads=['GPT', 'PP'], writes=[('G', 0)])
            softplus_parts(0, 1, 2)
            fw.op('dve', lambda v: v.scalar_tensor_tensor(out=G[3][:], in0=G[0][:], scalar=0.0, in1=G[2][:],
                                                         op0=ALU.min, op1=ALU.subtract),
                  reads=[('G', 0), ('G', 2)], writes=[('G', 3)])
            if stop == 'fox_g2':
                return
            cumsum(3, 4)
            fw.op('dve', lambda v: v.tensor_scalar(out=G[5][:], in0=G[4][:], scalar1=-1.0, scalar2=None, op0=ALU.mult),
                  reads=[('G', 4)], writes=[('G', 5)])
            if stop == 'fox_g':
                return
            build_aug(4, 0, 1)
            if stop == 'fox_a':
                return
            b = load_w8(Win, [(0, 0, 512)])
            for cc in range(4):
                for tb in range(4):
                    ps = proj_chunk(b, cc * 128, tb)
                    if cc < 2:
                        fw.op('act', lambda a, ps=ps, cc=cc, tb=tb: a.activation(out=A[0][:, cc, tbs(tb)], in_=PS[ps][:],
                                                                                func=AF.Copy, scale=0.125),
                              reads=[('PS', ps)], writes=qk_writes('q', cc, tb))
                    else:
                        fw.op('act', lambda a, ps=ps, cc=cc, tb=tb: a.activation(out=A[1][:, cc - 2, tbs(tb)], in_=PS[ps][:],
                                                                                func=AF.Copy),
                              reads=[('PS', ps)], writes=qk_writes('k', cc - 2, tb))

            def post_fox(h, tb, po):
                pb = 64 * (h % 2)
                r = tmpf()
                fw.op('dve', lambda v: v.reciprocal(out=T[r][pb:pb + 64, :], in_=PS[po][64 - pb:128 - pb, :]),
                      reads=[('PS', po)], writes=[('T', r)])
                fw.op('dve', lambda v: v.tensor_tensor(out=A[2][pb:pb + 64, h // 2, tbs(tb)], in0=PS[po][pb:pb + 64, :],
                                                      in1=T[r][pb:pb + 64, :], op=ALU.mult),
                      reads=[('PS', po), ('T', r)], writes=[('A', 2, h // 2, tb)])
            attention('softmax', std_qk, v_aug, 128, post_fox)
            out_proj(l, 0)
            if stop == 'fox':
                return
            proj_tm(Win, 1284, 1540, 8)
            fw.op('dve', lambda v: v.tensor_tensor(out=G[6][:], in0=GPT[:, :, 0:4], in1=bcast4(o['mib']), op=ALU.add),
                  reads=['GPT', 'PP'], writes=[('G', 6)])
            fw.op('dve', lambda v: v.tensor_tensor(out=G[0][:], in0=GPT[:, :, 4:8], in1=bcast4(o['mfb']), op=ALU.add),
                  reads=['GPT', 'PP'], writes=[('G', 0)])
            softplus_parts(0, 1, 2)
            fw.op('dve', lambda v: v.scalar_tensor_tensor(out=G[3][:], in0=G[0][:], scalar=0.0, in1=G[2][:],
                                                         op0=ALU.min, op1=ALU.subtract),
                  reads=[('G', 0), ('G', 2)], writes=[('G', 3)])
            cumsum(3, 4)
            fw.op('dve', lambda v: v.scalar_tensor_tensor(out=G[5][:], in0=G[6][:], scalar=math.log(0.125), in1=G[4][:],
                                                         op0=ALU.add, op1=ALU.subtract),
                  reads=[('G', 6), ('G', 4)], writes=[('G', 5)])
            build_aug(4, 0, 1)
            b = load_w8(Win, [(0, 1548, 256)])
            for cc in range(2):
                for tb in range(4):
                    ps = proj_chunk(b, cc * 128, tb)
                    fw.op('act', lambda a, ps=ps, cc=cc, tb=tb: a.activation(out=A[3][:, cc, tbs(tb)], in_=PS[ps][:],
                                                                            func=AF.Sigmoid),
                          reads=[('PS', ps)], writes=[('A', 3, cc, tb)])
            b = load_w8(Win, [(0, 772, 512)])
            for cc in range(4):
                for tb in range(4):
                    ps = proj_chunk(b, cc * 128, tb)
                    if cc < 2:
                        conv_silu(l, ps, o['mcw'], o['mcb'], cc, tb, A[0][:, cc, tbs(tb)], qk_writes('q', cc, tb))
                    else:
                        conv_silu(l, ps, o['mcw'], o['mcb'], cc, tb, A[1][:, cc - 2, tbs(tb)], qk_writes('k', cc - 2, tb))

            def post_mlstm(h, tb, po):
                pb = 64 * (h % 2)
                r = tmpf()
                fw.op('act', lambda a: a.activation(out=T[r][pb:pb + 64, :], in_=PS[po][64 - pb:128 - pb, :], func=AF.Abs),
                      reads=[('PS', po)], writes=[('T', r)])
                fw.op('dve', lambda v: v.tensor_scalar(out=T[r][pb:pb + 64, :], in0=T[r][pb:pb + 64, :], scalar1=1.0,
                                                      scalar2=None, op0=ALU.max),
                      reads=[('T', r)], writes=[('T', r)])
                fw.op('dve', lambda v: v.reciprocal(out=T[r][pb:pb + 64, :], in_=T[r][pb:pb + 64, :]),
                      reads=[('T', r)], writes=[('T', r)])
                t = tmpf()
                fw.op('dve', lambda v: v.tensor_tensor(out=T[t][pb:pb + 64, :], in0=PS[po][pb:pb + 64, :],
                                                      in1=T[r][pb:pb + 64, :], op=ALU.mult),
                      reads=[('PS', po), ('T', r)], writes=[('T', t)])
                return head_norm_gate(l, h, tb, t, o['mng'])
            attention('linear', std_qk, v_aug, 128, post_mlstm)
            out_proj(l, 1)
            if stop == 'mlstm':
                return
            proj_tm(Win, 2316, 0, 0)
            fw.dma('sp', lambda e: e.dma_start(out=AUG[:], in_=rad),
                   writes=[('AUG', s_, t_) for s_ in range(2) for t_ in range(4)])
            fw.op('dve', lambda v: v.tensor_copy(out=G[5][:].rearrange("p a b -> p (a b)"), in_=CF[:, CF_RS:CF_RS + 64]),
                  reads=['CF'], writes=[('G', 5)])
            b = load_w8(Win, [(0, 2572, 256)])
            for cc in range(2):
                for tb in range(4):
                    ps = proj_chunk(b, cc * 128, tb)
                    fw.op('act', lambda a, ps=ps, cc=cc, tb=tb: a.activation(out=A[3][:, cc, tbs(tb)], in_=PS[ps][:],
                                                                            func=AF.Silu),
                          reads=[('PS', ps)], writes=[('A', 3, cc, tb)])
            for which, base in (('q', 1804), ('k', 2060)):
                pieces = [(0, base, 256)]
                for hh in range(4):
                    pieces.append((256 + 64 * hh, base + 64 * hh + 32, 32))
                    pieces.append((256 + 64 * hh + 32, base + 64 * hh, 32))
                b = load_w8(Win, pieces)
                dst = A[0] if which == 'q' else A[1]
                for tb in range(4):
                    fw.dma('sp', [lambda e, tb=tb: e.dma_start(out=ROPE[:, 0, :], in_=cfd[:, CF_COS + tb * 512:CF_COS + (tb + 1) * 512]),
                                  lambda e, tb=tb: e.dma_start(out=ROPE[:, 1, :], in_=cfd[:, CF_SIN + tb * 512:CF_SIN + (tb + 1) * 512])],
                           writes=['ROPE'])
                    for cc in range(2):
                        p1 = proj_chunk(b, cc * 128, tb)
                        p2 = proj_chunk(b, 256 + cc * 128, tb)
                        t1 = tmpf()
                        t2 = tmpf()
                        fw.op('dve', lambda v, p1=p1, t1=t1: v.tensor_tensor(out=T[t1][:], in0=PS[p1][:], in1=ROPE[:, 0, :],
                                                                             op=ALU.mult),
                              reads=[('PS', p1), 'ROPE'], writes=[('T', t1)])
                        fw.op('dve', lambda v, p2=p2, t2=t2: v.tensor_tensor(out=T[t2][:], in0=PS[p2][:], in1=ROPE[:, 1, :],
                                                                             op=ALU.mult),
                              reads=[('PS', p2), 'ROPE'], writes=[('T', t2)])
                        fw.op('dve', lambda v, t1=t1, t2=t2, cc=cc, tb=tb, dst=dst: v.tensor_tensor(
                            out=dst[:, cc, tbs(tb)], in0=T[t1][:], in1=T[t2][:], op=ALU.add),
                            reads=[('T', t1), ('T', t2)], writes=qk_writes(which, cc, tb))

            def post_ret(h, tb, po):
                pb = 64 * (h % 2)
                t = tmpf()
                fw.op('act', lambda a: a.activation(out=T[t][pb:pb + 64, :], in_=PS[po][pb:pb + 64, :], func=AF.Copy),
                      reads=[('PS', po)], writes=[('T', t)])
                return head_norm_gate(l, h, tb, t, o['rng'])
            attention('linear', std_qk, v_aug, 128, post_ret)
            out_proj(l, 2)
            if stop == 'ret':
                return
            proj_tm(Win, None, 3596, 4)
            fw.op('dve', lambda v: v.tensor_tensor(out=G[0][:], in0=GPT[:, :, 0:4], in1=bcast4(o['sdtb']), op=ALU.add),
                  reads=['GPT', 'PP'], writes=[('G', 0)])
            softplus_parts(0, 1, 2)
            fw.op('dve', lambda v: v.scalar_tensor_tensor(out=G[3][:], in0=G[0][:], scalar=0.0, in1=G[2][:],
                                                         op0=ALU.max, op1=ALU.add),
                  reads=[('G', 0), ('G', 2)], writes=[('G', 3)])
            fw.op('act', lambda a: a.activation(out=AEXP[:], in_=PP[:, o['sAl']:o['sAl'] + 4], func=AF.Exp),
                  reads=['PP'], writes=['AEXP'])
            fw.op('dve', lambda v: v.scalar_tensor_tensor(out=G[6][:], in0=G[3][:], scalar=-1.0,
                                                         in1=AEXP[:].unsqueeze(1).to_broadcast([128, 16, 4]),
                                                         op0=ALU.mult, op1=ALU.mult),
                  reads=[('G', 3), 'AEXP'], writes=[('G', 6)])
            cumsum(6, 4)
            fw.op('act', lambda a: a.activation(out=G[2][:], in_=G[3][:], func=AF.Ln), reads=[('G', 3)], writes=[('G', 2)])
            fw.op('dve', lambda v: v.tensor_tensor(out=G[5][:], in0=G[2][:], in1=G[4][:], op=ALU.subtract),
                  reads=[('G', 2), ('G', 4)], writes=[('G', 5)])
            build_aug(4, 0, 1)
            if stop == 'fox_ssd_g':
                return
            b = load_w8(Win, [(0, 2828, 256)])
            for cc in range(2):
                for tb in range(4):
                    ps = proj_chunk(b, cc * 128, tb)
                    fw.op('act', lambda a, ps=ps, cc=cc, tb=tb: a.activation(out=A[3][:, cc, tbs(tb)], in_=PS[ps][:],
                                                                            func=AF.Silu),
                          reads=[('PS', ps)], writes=[('A', 3, cc, tb)])
            b = load_w8(Win, [(0, 3084, 512)])
            dsts = [(0, 1), (1, 1), (1, 0), (0, 0)]
            for cc in range(4):
                ai, ac = dsts[cc]
                for tb in range(4):
                    ps = proj_chunk(b, cc * 128, tb)
                    wk = [('A', ai, ac, tb)]
                    if cc == 2:
                        wk += [qk_key(hh, 'k', tb) for hh in range(4)]
                    if cc == 3:
                        wk += [qk_key(hh, 'q', tb) for hh in range(4)]
                    conv_silu(l, ps, o['scw'], o['scb'], cc, tb, A[ai][:, ac, tbs(tb)], wk)
            for i in range(16):
                half = i % 2
                fw.ops('pe', [lambda t, i=i, c=c, half=half: t.transpose(
                    out=PSB[:, half * 512 + c * 128:half * 512 + (c + 1) * 128],
                    in_=A[c][:, 1, i * 128:(i + 1) * 128], identity=IDENT) for c in range(2)],
                    reads=[('A', 0, 1, i // 4), ('A', 1, 1, i // 4), 'CB'], writes=[('PS', 7)])
                for u in range(2):
                    fw.op('act', lambda a, i=i, half=half, u=u: a.activation(
                        out=v_pair(i, u),
                        in_=PSB[:, half * 512 + 128 * u:half * 512 + 128 * u + 128].rearrange("p (a b) -> p a b", b=64),
                        func=AF.Copy), reads=[('PS', 7)], writes=[('V', i)])

            if stop == 'fox_ssd_p':
                return

            def ssd_qk(h):
                g = h // 2
                return A[0][64 * g:64 * g + 64, 0, :], A[1][64 * g:64 * g + 64, 0, :]
            ssd_tiles = {}

            def pre_ssd(tb):
                pass

            def post_ssd(h, tb, po):
                if os.environ.get('SKIP_SSD_POST') == '1':
                    return
                pb = 64 * (h % 2)
                c = h // 2
                xh = A[c][pb:pb + 64, 1, tbs(tb)]
                fw.op('dve', lambda v: v.scalar_tensor_tensor(out=ROPE[pb:pb + 64, c, :], in0=xh,
                                                             scalar=PP[pb:pb + 64, o['sD'] + c:o['sD'] + c + 1],
                                                             in1=PS[po][pb:pb + 64, :], op0=ALU.mult, op1=ALU.add),
                      reads=[('A', c, 1, tb), 'PP', ('PS', po)], writes=['ROPE'])
                fw.op('dve', lambda v: v.tensor_tensor(out=ROPE[pb:pb + 64, c, :], in0=ROPE[pb:pb + 64, c, :],
                                                      in1=A[3][pb:pb + 64, c, tbs(tb)], op=ALU.mult),
                      reads=['ROPE', ('A', 3, c, tb)], writes=['ROPE'])
                if h == 3 and os.environ.get('SKIP_SSD_POST') != '2':
                    y0, y1 = 0, 1
                    ykeys = ['ROPE']
                    for c2, yy in enumerate((y0, y1)):
                        fw.op('act', lambda a, yy=yy, c2=c2: a.activation(out=A[2][:, c2, tbs(tb)], in_=ROPE[:, yy, :],
                                                                          func=AF.Square),
                              reads=ykeys, writes=[('A', 2, c2, tb)])
                    return lambda: post_ssd2(tb, ykeys, y0, y1)

            def post_ssd2(tb, ykeys, y0, y1):
                if True:
                    pn = psn()
                    fw.ops('pe', [mm(PS[pn][:, :], lhsT=ONESB, rhs=A[2][:, c2, tbs(tb)], start=(c2 == 0), stop=(c2 == 1))
                                  for c2 in range(2)], reads=[('A', 2, 0, tb), ('A', 2, 1, tb), 'CB'], writes=[('PS', pn)])
                    r = tmpf()
                    fw.op('act', lambda a: a.activation(out=T[r][:], in_=PS[pn][:], func=AF.Sqrt, scale=1.0 / 256.0,
                                                        bias=EPSB[:, 0:1]), reads=[('PS', pn), 'EPSB'], writes=[('T', r)])
                    fw.op('dve', lambda v: v.reciprocal(out=T[r][:], in_=T[r][:]), reads=[('T', r)], writes=[('T', r)])
                    for c2, yy in enumerate((y0, y1)):
                        fw.op('dve', lambda v, c2=c2, yy=yy: v.scalar_tensor_tensor(
                            out=A[2][:, c2, tbs(tb)], in0=ROPE[:, yy, :], scalar=PP[:, o['sng'] + c2:o['sng'] + c2 + 1],
                            in1=T[r][:], op0=ALU.mult, op1=ALU.mult),
                            reads=ykeys + [('T', r), 'PP'], writes=[('A', 2, c2, tb)])
            attention('linear', ssd_qk, v_aug, 128, post_ssd, share_qk=True)
            out_proj(l, 3)

        AEXP = fw.sb("AEXP", [128, 4], F32)

        done = False
        for l in range(L):
            if stop == 'h0':
                norm_mod(l, 0)
                break
            norm_prep(l, 0)
            ffn(l, 0, pre={0: [lambda l=l: norm_tb(l, 0, 0), lambda l=l: norm_tb(l, 0, 1)],
                           1: [lambda l=l: norm_tb(l, 0, 2)], 2: [lambda l=l: norm_tb(l, 0, 3)]})
            if stop == 'ffn1':
                break
            norm_prep(l, 1)
            mixer(l)
            if stop is not None and (stop in ('fox', 'mlstm', 'ret', 'mix') or stop.startswith('fox_')):
                break
            norm_prep(l, 2)
            ffn(l, 1, pre={0: [lambda l=l: norm_tb(l, 2, 0), lambda l=l: norm_tb(l, 2, 1)],
                           1: [lambda l=l: norm_tb(l, 2, 2)], 2: [lambda l=l: norm_tb(l, 2, 3)]})
        out_toks = []
        if stop == 'h0':
            for c in range(8):
                for tb in range(4):
                    t = tmpf()
                    fw.op('dve', lambda v, c=c, tb=tb, t=t: v.tensor_copy(out=T[t][:], in_=H[:, c, tbs(tb)]),
                          reads=[('H', c, tb)], writes=[('T', t)])
                    out_toks.append(fw.dma('sp', lambda e, c=c, tb=tb, t=t: e.dma_start(
                        out=yT[c * 128:(c + 1) * 128, tbs(tb)], in_=T[t][:]), reads=[('T', t)]))
        elif stop is not None:
            for c in range(8):
                out_toks.append(fw.dma('sp', lambda e, c=c: e.dma_start(out=yT[c * 128:(c + 1) * 128, :], in_=X[:, c, :]),
                                       reads=[('X', c, tb) for tb in range(4)]))
        else:
            SQv = A[0][:].rearrange("p a (b n) -> p (a b) n", n=512)
            a0keys = [('A', 0, c, t) for c in range(2) for t in range(4)]
            for tb in range(4):
                fw.op('act', lambda a, tb=tb: a.activation(out=SQv, in_=X[:, :, tbs(tb)], func=AF.Square),
                      reads=[('X', c, tb) for c in range(8)], writes=a0keys)
                ps = psn()
                fw.ops('pe', [mm(PS[ps][:, :], lhsT=ONESB, rhs=SQv[:, c, :], start=(c == 0), stop=(c == 7))
                              for c in range(8)], reads=a0keys + ['CB'], writes=[('PS', ps)])
                fw.op('act', lambda a, ps=ps: a.activation(out=RSTD, in_=PS[ps][:], func=AF.Sqrt,
                                                           scale=1.0 / 1024.0, bias=EPSB[:, 0:1]),
                      reads=[('PS', ps), 'EPSB'], writes=['CS'])
                fw.op('dve', lambda v: v.reciprocal(out=RSTD, in_=RSTD), reads=['CS'], writes=['CS'])
                for c in range(8):
                    fw.op('dve', lambda v, c=c, tb=tb: v.scalar_tensor_tensor(
                        out=X[:, c, tbs(tb)], in0=X[:, c, tbs(tb)], scalar=PP[:, PPL['fg'] + c:PPL['fg'] + c + 1],
                        in1=RSTD, op0=ALU.mult, op1=ALU.mult),
                        reads=[('X', c, tb), 'CS', 'PP'], writes=[('X', c, tb)])
            for c in range(8):
                out_toks.append(fw.dma('sp', lambda e, c=c: e.dma_start(out=yT[c * 128:(c + 1) * 128, :], in_=X[:, c, :]),
                                       reads=[('X', c, tb) for tb in range(4)]))
        fw.wait_all('sp', out_toks)
        fw.emit()
        print("kernel build: instructions", fw.n_ins, "waits", fw.n_wait, {e: fw.cnt[e] for e in ENGS}, flush=True)
    return nc


_CONSTS = None


def make_in_maps(inp, cores, NLW=4):
    global _CONSTS
    if _CONSTS is None:
        _CONSTS = make_consts()
    cb, cf, ra = _CONSTS
    f32 = lambda a: np.ascontiguousarray(np.asarray(a, np.float32))
    shared = {k: f32(inp[k][:NLW]) for k in ('ada_w', 'ffn1_w13', 'ffn1_w2', 'ffn2_w13', 'ffn2_w2', 'w_in', 'w_out')}
    maps = []
    for b in cores:
        m = dict(shared)
        m['xT'] = np.ascontiguousarray(np.asarray(inp['x'][b], np.float32).T)
        m['pp'] = pack_pp(inp, b)
        m['cb'] = cb
        m['cf'] = cf
        m['retaug'] = ra
        maps.append(m)
    return maps


def kernel(**inputs):
    inp = {k: np.asarray(v) for k, v in inputs.items()}
    nc = build(L=4)
    maps = make_in_maps(inp, list(range(8)))
    res = run_bass_kernel_spmd(nc, maps, core_ids=list(range(8)))
    out = np.stack([np.ascontiguousarray(res.results[b]['yT'].T) for b in range(8)], axis=0)
    return out.astype(np.float32)
```

```python
import math
import os
import numpy as np
import ml_dtypes
from contextlib import ExitStack
import concourse.bass as bass
import concourse.mybir as mybir
from concourse.bass_utils import run_bass_kernel_spmd

F32 = mybir.dt.float32
BF16 = mybir.dt.bfloat16
AF = mybir.ActivationFunctionType
ALU = mybir.AluOpType

ENGS = ['pe', 'act', 'dve', 'pool', 'sp']
N_DMA_SEMS = 12
S = 2048
NTB = 4
EPS = 1e-6
NEG = -30000.0


class Fw:
    def __init__(self, nc, ctx, self_sync=True):
        self.nc = nc
        self.ctx = ctx
        self.self_sync = self_sync
        self.q = {e: [] for e in ENGS}
        self.cnt = {e: 0 for e in ENGS}
        self.semobj = {}
        for e in ['pe', 'act', 'dve', 'pool']:
            self.semobj['s_' + e] = ctx.enter_context(nc.semaphore('s_' + e))
        for p in 'dw':
            for i in range(N_DMA_SEMS):
                self.semobj['%s%d' % (p, i)] = ctx.enter_context(nc.semaphore('%s%d' % (p, i)))
        self.dcnt = {p: [0] * N_DMA_SEMS for p in 'dw'}
        self.dlast = {p: [None] * N_DMA_SEMS for p in 'dw'}
        self.dnext = {p: 0 for p in 'dw'}
        self.last_w = {}
        self.readers = {}
        self.seen = {e: {} for e in ENGS}
        self.n_wait = 0
        self.n_ins = 0

    def sb(self, name, shape, dt):
        return self.ctx.enter_context(self.nc.sbuf_tensor(name, shape, dt))

    def ps(self, name, shape, dt=F32):
        return self.ctx.enter_context(self.nc.psum_tensor(name, shape, dt))

    def _deps(self, e, reads, writes):
        toks = []
        for k in reads:
            t = self.last_w.get(k)
            if t is not None:
                toks.append(t)
            if isinstance(k, tuple) and k[0] == 'PS':
                toks.extend(self.readers.get(k, ()))
        for k in writes:
            t = self.last_w.get(k)
            if t is not None:
                toks.append(t)
            toks.extend(self.readers.get(k, ()))
        need = {}
        for (sk, val, te) in toks:
            if te == e and (e == 'pe' or not self.self_sync):
                continue
            if self.seen[e].get(sk, 0) >= val:
                continue
            if need.get(sk, 0) < val:
                need[sk] = val
        for sk, val in need.items():
            self.seen[e][sk] = val
        return list(need.items())

    def _commit(self, tok, reads, writes):
        for k in reads:
            lst = self.readers.setdefault(k, [])
            lst[:] = [t for t in lst if t[0] != tok[0]]
            lst.append(tok)
        for k in writes:
            self.last_w[k] = tok
            self.readers[k] = []

    def ops(self, e, fns, reads=(), writes=()):
        if not isinstance(fns, (list, tuple)):
            fns = [fns]
        waits = self._deps(e, reads, writes)
        self.cnt[e] += 1
        tok = ('s_' + e, self.cnt[e], e)
        n = len(fns)
        for i, fn in enumerate(fns):
            self.q[e].append((fn, waits if i == 0 else [], ('s_' + e, 1) if i == n - 1 else None))
        self._commit(tok, reads, writes)
        self.n_ins += n
        self.n_wait += len(waits)
        return tok

    op = ops

    def dma(self, e, fns, reads=(), writes=()):
        if not isinstance(fns, (list, tuple)):
            fns = [fns]
        p = 'w' if e == 'pool' else 'd'
        i = self.dnext[p]
        self.dnext[p] = (i + 1) % N_DMA_SEMS
        sk = '%s%d' % (p, i)
        waits = self._deps(e, reads, writes)
        prev = self.dlast[p][i]
        if prev is not None and self.seen[e].get(sk, 0) < prev[1]:
            waits.append((sk, prev[1]))
            self.seen[e][sk] = prev[1]
        self.dcnt[p][i] += 16 * len(fns)
        tok = (sk, self.dcnt[p][i], 'dma')
        self.dlast[p][i] = tok
        for j, fn in enumerate(fns):
            self.q[e].append((fn, waits if j == 0 else [], (sk, 16)))
        self._commit(tok, reads, writes)
        self.n_ins += len(fns)
        self.n_wait += len(waits)
        return tok

    def wait_all(self, e, toks):
        waits = []
        for (sk, val, te) in toks:
            if self.seen[e].get(sk, 0) < val:
                waits.append((sk, val))
                self.seen[e][sk] = val
        self.q[e].append((None, waits, None))

    def emit(self):
        nc = self.nc
        with nc.Block() as block:
            def run(e):
                def body(engine):
                    for (fn, waits, inc) in self.q[e]:
                        for (sk, val) in waits:
                            engine.wait_ge(self.semobj[sk], val)
                        if fn is None:
                            continue
                        ins = fn(engine)
                        if inc is not None:
                            ins.then_inc(self.semobj[inc[0]], inc[1])
                return body
            block.sync(run('sp'))
            block.scalar(run('act'))
            block.vector(run('dve'))
            block.gpsimd(run('pool'))
            block.tensor(run('pe'))


def _pp_layout():
    off = [0]

    def al(n):
        o = off[0]
        off[0] += n
        return o
    lay = {'c': al(8), 'fg': al(8), 'L': []}
    for l in range(4):
        lay['L'].append(dict(ada_b=al(72), ng=al(24), mcw=al(16), mcb=al(4), scw=al(16), scb=al(4),
                             mng=al(2), rng=al(2), sng=al(2), sD=al(2),
                             ffb=al(4), mib=al(4), mfb=al(4), sdtb=al(4), sAl=al(4)))
    lay['n'] = off[0]
    return lay


PPL = _pp_layout()
CB_ID, CB_MASK, CB_ONES, CB_SEL1, CB_SEL2, NCB = 0, 128, 128 + 960, 128 + 960 + 128, 128 + 960 + 256, 128 + 960 + 384
CF_TRI, CF_ONES, CF_RS, CF_COS, CF_SIN, NCF = 0, 128, 256, 320, 320 + 2048, 320 + 4096


def col(w, n):
    return np.ascontiguousarray(np.asarray(w, np.float32).reshape(n, 128).T)


def pack_pp(inp, b):
    pp = np.zeros((128, PPL['n']), np.float32)
    pp[:, PPL['c']:PPL['c'] + 8] = col(inp['c'][b], 8)
    pp[:, PPL['fg']:PPL['fg'] + 8] = col(inp['final_g'], 8)
    for l in range(4):
        o = PPL['L'][l]
        pp[:, o['ada_b']:o['ada_b'] + 72] = col(inp['ada_b'][l], 72)
        for i in range(3):
            pp[:, o['ng'] + 8 * i:o['ng'] + 8 * i + 8] = col(inp['norm_g'][l, i], 8)
        for j in range(4):
            pp[:, o['mcw'] + j:o['mcw'] + 16:4] = col(inp['mlstm_conv_w'][l, j], 4)
            pp[:, o['scw'] + j:o['scw'] + 16:4] = col(inp['ssd_conv_w'][l, j], 4)
        pp[:, o['mcb']:o['mcb'] + 4] = col(inp['mlstm_conv_b'][l], 4)
        pp[:, o['scb']:o['scb'] + 4] = col(inp['ssd_conv_b'][l], 4)
        pp[:, o['mng']:o['mng'] + 2] = col(inp['mlstm_norm_g'][l], 2)
        pp[:, o['rng']:o['rng'] + 2] = col(inp['ret_norm_g'][l], 2)
        pp[:, o['sng']:o['sng'] + 2] = col(inp['ssd_norm_g'][l], 2)
        pp[:, o['sD']:o['sD'] + 2] = col(np.repeat(inp['ssd_D'][l], 64), 2)
        for nm, key in (('ffb', 'fox_fb'), ('mib', 'mlstm_ib'), ('mfb', 'mlstm_fb'), ('sdtb', 'ssd_dt_bias'),
                        ('sAl', 'ssd_A_log')):
            pp[:, o[nm]:o[nm] + 4] = np.broadcast_to(inp[key][l][None, :], (128, 4))
    return pp


def split3(x):
    x = np.asarray(x, np.float32)
    hi = x.astype(ml_dtypes.bfloat16)
    r = x - hi.astype(np.float32)
    mid = r.astype(ml_dtypes.bfloat16)
    r2 = r - mid.astype(np.float32)
    lo = r2.astype(ml_dtypes.bfloat16)
    return hi, mid, lo


def make_consts():
    cb = np.zeros((128, NCB), np.float32)
    cb[:, CB_ID:CB_ID + 128] = np.eye(128)
    kk = (np.arange(128) % 64)[:, None]
    uu = np.arange(960)[None, :]
    cb[:, CB_MASK:CB_MASK + 960] = np.where(kk <= uu - 448, 0.0, NEG)
    for k in range(128):
        cb[k, CB_SEL1 + (k % 64)] = 1.0
        cb[k, CB_SEL2 + (k % 64) + 64] = 1.0
    cb[:, CB_ONES:CB_ONES + 128] = 1.0
    cb = cb.astype(ml_dtypes.bfloat16)
    cf = np.zeros((128, NCF), np.float32)
    cf[:, CF_TRI:CF_TRI + 128] = (np.arange(128)[:, None] <= np.arange(128)[None, :]).astype(np.float32)
    cf[:, CF_ONES:CF_ONES + 128] = 1.0
    d = 64
    inv = (1.0 / (np.float32(10000.0) ** (np.arange(0, d, 2, dtype=np.float32) / np.float32(d)))).astype(np.float32)
    ang = np.arange(S, dtype=np.float32)[:, None] * inv[None, :]
    cos, sin = np.cos(ang).astype(np.float32), np.sin(ang).astype(np.float32)
    for p in range(128):
        i = p % 32
        cf[p, CF_COS:CF_COS + S] = cos[:, i]
        cf[p, CF_SIN:CF_SIN + S] = (-sin[:, i]) if (p % 64) < 32 else sin[:, i]
    ra = np.zeros((128, 2, S), ml_dtypes.bfloat16)
    t = np.arange(S, dtype=np.float64)
    rs = np.zeros((128, 16, 4), np.float32)
    for h in range(4):
        lg = np.log(np.float32(1.0) - np.float32(2.0) ** np.float32(-5.0 - h)).astype(np.float32)
        T = (t * np.float64(lg)).astype(np.float32)
        Sv = (-(t * np.float64(lg)) + math.log(0.125)).astype(np.float32)
        for r, v in enumerate(split3(T)):
            ra[64 * (h % 2) + r, h // 2] = v
        rs[:, :, h] = Sv.reshape(16, 128).T
    cf[:, CF_RS:CF_RS + 64] = rs.reshape(128, 64)
    return cb, cf, ra


def mm(out, lhsT, rhs, start, stop, **kw):
    return lambda t: t.matmul(out, lhsT=lhsT, rhs=rhs, start=start, stop=stop, **kw)


def build(L=4, stop=None, self_sync=True, NLW=4):
    nc = bass.Bass("TRN2", target_bir_lowering=False)

    def D(name, shape, dt, kind="ExternalInput"):
        return nc.dram_tensor(name, shape, dt, kind=kind).ap()
    xT = D("xT", [1024, S], F32)
    ppd = D("pp", [128, PPL['n']], F32)
    cbd = D("cb", [128, NCB], BF16)
    cfd = D("cf", [128, NCF], F32)
    rad = D("retaug", [128, 2, S], BF16)
    ada_w = D("ada_w", [NLW, 1024, 9216], F32)
    w13d = [D("ffn1_w13", [NLW, 1024, 5632], F32), D("ffn2_w13", [NLW, 1024, 5632], F32)]
    w2d = [D("ffn1_w2", [NLW, 2816, 1024], F32), D("ffn2_w2", [NLW, 2816, 1024], F32)]
    w_in = D("w_in", [NLW, 1024, 3600], F32)
    w_out = D("w_out", [NLW, 1024, 1024], F32)
    yT = D("yT", [1024, S], F32, kind="ExternalOutput")

    with ExitStack() as ctx:
        fw = Fw(nc, ctx, self_sync=self_sync)
        X = fw.sb("X", [128, 8, S], F32)
        H = fw.sb("H", [128, 8, S], BF16)
        W8 = [fw.sb("W8_%d" % i, [128, 8, 512], BF16) for i in range(2)]
        W2 = [fw.sb("W2_%d" % i, [128, 2, 1024], BF16) for i in range(2)]
        A = [fw.sb("A_%d" % i, [128, 2, S], BF16) for i in range(4)]
        AUG = fw.sb("AUG", [128, 2, S], BF16)
        V = fw.sb("V", [128, 16, 384], BF16)
        SPL = fw.sb("SPL", [128, 16, 128], BF16)
        NT = 5
        T = [fw.sb("T_%d" % i, [128, 512], F32) for i in range(NT)]
        NPT = 4
        PTB = [fw.sb("PT_%d" % i, [128, 512], BF16) for i in range(NPT)]
        CS = fw.sb("CS", [128, 516], F32)
        RSTD = CS[:, 0:512]
        ROPE = fw.sb("ROPE", [128, 2, 512], F32)
        PP = fw.sb("PP", [128, PPL['n']], F32)
        CB = fw.sb("CB", [128, NCB], BF16)
        CF = fw.sb("CF", [128, 320], F32)
        COND = fw.sb("COND", [128, 4, 72], F32)
        CA2 = fw.sb("CA2", [128, 8, 2], F32)
        AB = fw.sb("AB", [128, 8], F32)
        GT8 = fw.sb("GT8", [128, 8], F32)
        GPT = fw.sb("GPT", [128, 16, 8], F32)
        G = [fw.sb("G_%d" % i, [128, 16, 4], F32) for i in range(7)]
        PRE = fw.sb("PRE", [128, 16, 4], F32)
        NPS = 7
        PS = [fw.ps("PS_%d" % i, [128, 512], F32) for i in range(NPS)]
        PSB = fw.ps("PSB", [128, 1024], BF16)
        PS.append(PSB[:].bitcast(F32))
        ROTB = [0, 1, 2, 3, 4, 7]
        IDENT = CB[:, CB_ID:CB_ID + 128]
        ONESB = CB[:, CB_ONES:CB_ONES + 128]
        TRI = CF[:, CF_TRI:CF_TRI + 128]
        ONESF = CF[:, CF_ONES:CF_ONES + 128]

        rot = {'ps': 0, 't': 0, 'pt': 0, 'w8': 0, 'w2': 0}

        def nxt(k, n):
            v = rot[k]
            rot[k] = (v + 1) % n
            return v

        rot['po'] = 0

        def psn():
            return ROTB[nxt('ps', len(ROTB))]

        def pon():
            return NPS - 2 + nxt('po', 2)

        def tmpf():
            return nxt('t', NT)

        def ptn():
            return nxt('pt', NPT)

        def tbs(tb):
            return slice(tb * 512, (tb + 1) * 512)

        fw.dma('sp', lambda e: e.dma_start(out=PP[:], in_=ppd), writes=['PP'])
        fw.dma('sp', lambda e: e.dma_start(out=CB[:], in_=cbd), writes=['CB'])
        fw.dma('sp', lambda e: e.dma_start(out=CF[:], in_=cfd[:, 0:320]), writes=['CF'])
        for c in range(8):
            fw.dma('act', lambda e, c=c: e.dma_start(out=X[:, c, :], in_=xT[c * 128:(c + 1) * 128, :]),
                   writes=[('X', c, tb) for tb in range(4)])
        fw.op('pool', lambda g: g.memset(V[:, :, 64:128], 1.0), writes=[('V', i) for i in range(16)])
        fw.op('pool', lambda g: g.memset(V[:, :, 256:320], 1.0), writes=[('V', i) for i in range(16)])
        print("sbuf bytes remaining", nc.sbuf_bytes_remaining, flush=True)
        fw.op('pool', lambda g: g.memset(SPL[:], 0.0), writes=[('SPL', i) for i in range(16)])
        fw.op('pool', lambda g: g.memset(CS[:], 0.0), writes=['CS'])
        for j in range(2):
            fw.op('act', lambda a, j=j: a.activation(out=CA2[:, :, j], in_=PP[:, PPL['c']:PPL['c'] + 8], func=AF.Silu),
                  reads=['PP'], writes=['CA2'])
        Hf = H[:].bitcast(F32)
        CPSI = NPS - 1
        gi = 0
        for l in range(1):
            awl = ada_w[l].rearrange("(k p) n -> p k n", p=128)
            for g in range(18):
                b = gi % 2
                gi += 1
                keys = [('H', k, 2 * b + u) for k in range(8) for u in range(2)]
                fw.dma('sp', lambda e, b=b, g=g, awl=awl: e.dma_start(out=Hf[:, :, b * 512:(b + 1) * 512],
                                                                     in_=awl[:, :, g * 512:(g + 1) * 512]),
                       writes=keys)
                fns = []
                for j in range(4):
                    n = g * 4 + j
                    for k in range(8):
                        fns.append(mm(PS[CPSI][:, 2 * n:2 * n + 2],
                                      lhsT=Hf[:, k, b * 512 + j * 128:b * 512 + (j + 1) * 128],
                                      rhs=CA2[:, k, :], start=(k == 0), stop=(k == 7)))
                fw.ops('pe', fns, reads=keys + ['CA2'], writes=[('PS', CPSI)])
            ab = PPL['L'][l]['ada_b']
            fw.op('dve', lambda v, l=l, ab=ab: v.tensor_tensor(out=COND[:, l, :], in0=PS[CPSI][:, 0:144:2],
                                                             in1=PP[:, ab:ab + 72], op=ALU.add),
                  reads=[('PS', CPSI), 'PP'], writes=[('COND', l)])

        def cond_piece(l, p):
            bi = 2 + p % 2
            Af8 = A[bi][:].bitcast(F32).rearrange("p a (k n) -> p (a k) n", n=256)
            keys = [('A', bi, c, t) for c in range(2) for t in range(4)]
            awl = ada_w[l].rearrange("(k p) n -> p k n", p=128)
            fw.dma('sp', lambda e: e.dma_start(out=Af8, in_=awl[:, :, p * 256:(p + 1) * 256]), writes=keys)
            fns = []
            for j in range(2):
                n = 2 * p + j
                for k in range(8):
                    fns.append(mm(PS[CPSI][:, 2 * n:2 * n + 2], lhsT=Af8[:, k, j * 128:(j + 1) * 128], rhs=CA2[:, k, :],
                                  start=(k == 0), stop=(k == 7)))
            fw.ops('pe', fns, reads=keys + ['CA2'], writes=[('PS', CPSI)])

        def cond_finish(l):
            ab = PPL['L'][l]['ada_b']
            fw.op('dve', lambda v: v.tensor_tensor(out=COND[:, l, :], in0=PS[CPSI][:, 0:144:2],
                                                  in1=PP[:, ab:ab + 72], op=ALU.add),
                  reads=[('PS', CPSI), 'PP'], writes=[('COND', l)])

        def norm_prep(l, i):
            o = PPL['L'][l]
            sc0 = (3 * i + 1) * 8
            fw.op('dve', lambda v: v.scalar_tensor_tensor(out=AB[:], in0=COND[:, l, sc0:sc0 + 8], scalar=1.0,
                                                         in1=PP[:, o['ng'] + 8 * i:o['ng'] + 8 * i + 8],
                                                         op0=ALU.add, op1=ALU.mult),
                  reads=[('COND', l), 'PP'], writes=['AB'])

        def norm_tb(l, i, tb):
            sh0 = (3 * i) * 8
            SQv = A[3][:].rearrange("p a (b n) -> p (a b) n", n=512)
            sqkeys = [('A', 3, c, t) for c in range(2) for t in range(4)]
            fw.op('act', lambda a: a.activation(out=SQv, in_=X[:, :, tbs(tb)], func=AF.Square),
                  reads=[('X', c, tb) for c in range(8)], writes=sqkeys)
            ps = psn()
            fw.ops('pe', [mm(PS[ps][:, :], lhsT=ONESB, rhs=SQv[:, c, :], start=(c == 0), stop=(c == 7))
                          for c in range(8)], reads=sqkeys + ['CB'], writes=[('PS', ps)])
            fw.op('act', lambda a: a.activation(out=RSTD, in_=PS[ps][:], func=AF.Sqrt,
                                                scale=1.0 / 1024.0, bias=EPSB[:, 0:1]),
                  reads=[('PS', ps), 'EPSB'], writes=['CS'])
            fw.op('dve', lambda v: v.reciprocal(out=RSTD, in_=RSTD), reads=['CS'], writes=['CS'])
            for c in range(8):
                t2 = tmpf()
                fw.op('dve', lambda v, c=c, t2=t2: v.tensor_tensor(out=T[t2][:], in0=X[:, c, tbs(tb)],
                                                                   in1=RSTD, op=ALU.mult),
                      reads=[('X', c, tb), 'CS'], writes=[('T', t2)])
                fw.op('act', lambda a, c=c, t2=t2: a.activation(out=H[:, c, tbs(tb)], in_=T[t2][:],
                                                                func=AF.Identity, scale=AB[:, c:c + 1],
                                                                bias=COND[:, l, sh0 + c:sh0 + c + 1]),
                      reads=[('T', t2), 'AB', ('COND', l)], writes=[('H', c, tb)])

        def norm_mod(l, i):
            norm_prep(l, i)
            for tb in range(4):
                norm_tb(l, i, tb)

        EPSB = fw.sb("EPSB", [128, 1], F32)
        fw.op('pool', lambda g: g.memset(EPSB[:], EPS), writes=['EPSB'])

        def emit_w2(b, ai, tb):
            for m in range(8):
                ps = psn()
                fw.ops('pe', [mm(PS[ps][:, :], lhsT=W2[b][:, c, m * 128:(m + 1) * 128], rhs=A[ai][:, c, tbs(tb)],
                                 start=(c == 0), stop=(c == 1)) for c in range(2)],
                       reads=[('W2', b), ('A', ai, 0, tb), ('A', ai, 1, tb)], writes=[('PS', ps)])
                fw.op('dve', lambda v, ps=ps, m=m, tb=tb: v.scalar_tensor_tensor(
                    out=X[:, m, tbs(tb)], in0=PS[ps][:], scalar=GT8[:, m:m + 1], in1=X[:, m, tbs(tb)],
                    op0=ALU.mult, op1=ALU.add),
                    reads=[('PS', ps), 'GT8', ('X', m, tb)], writes=[('X', m, tb)])

        def set_gate(l, part, scale):
            fw.op('dve', lambda v: v.tensor_scalar(out=GT8[:], in0=COND[:, l, part * 8:part * 8 + 8], scalar1=scale,
                                                  scalar2=None, op0=ALU.mult),
                  reads=[('COND', l)], writes=['GT8'])

        def ffn(l, f, pre=None):
            Wa = w13d[f][l].rearrange("(k p) n -> p k n", p=128)
            Wb = w2d[f][l].rearrange("(k p) n -> p k n", p=128)
            set_gate(l, 2 if f == 0 else 8, 0.5)
            prev = None
            for j in range(int(os.environ.get('FFN_NJ', '11'))):
                b = nxt('w8', 2)
                fw.dma('pool', [lambda e, b=b, j=j: e.dma_start(out=W8[b][:, :, 0:256], in_=Wa[:, :, j * 256:(j + 1) * 256]),
                                lambda e, b=b, j=j: e.dma_start(out=W8[b][:, :, 256:512],
                                                                in_=Wa[:, :, 2816 + j * 256:2816 + (j + 1) * 256])],
                       writes=[('W8', b)])
                b2 = nxt('w2', 2)
                fw.dma('pool', lambda e, b2=b2, j=j: e.dma_start(out=W2[b2][:], in_=Wb[:, 2 * j:2 * j + 2, :]),
                       writes=[('W2', b2)])
                ai = j % 2
                for tb in range(4):
                    for fn_ in (pre or {}).get(4 * j + tb, ()):
                        fn_()
                    if f == 1 and l + 1 < L and 4 * j + tb < 36:
                        cond_piece(l + 1, 4 * j + tb)
                    pss = []
                    for q in range(4):
                        ps = psn()
                        pss.append(ps)
                        fw.ops('pe', [mm(PS[ps][:, :], lhsT=W8[b][:, k, q * 128:(q + 1) * 128], rhs=H[:, k, tbs(tb)],
                                         start=(k == 0), stop=(k == 7)) for k in range(8)],
                               reads=[('W8', b)] + [('H', k, tb) for k in range(8)], writes=[('PS', ps)])
                    for c in range(2):
                        t = tmpf()
                        fw.op('act', lambda a, t=t, p=pss[2 + c]: a.activation(out=T[t][:], in_=PS[p][:], func=AF.Silu),
                              reads=[('PS', pss[2 + c])], writes=[('T', t)])
                        fw.op('dve', lambda v, t=t, p=pss[c], c=c, tb=tb, ai=ai: v.tensor_tensor(
                            out=A[ai][:, c, tbs(tb)], in0=PS[p][:], in1=T[t][:], op=ALU.mult),
                            reads=[('PS', pss[c]), ('T', t)], writes=[('A', ai, c, tb)])
                    if prev is not None:
                        emit_w2(*prev)
                    prev = (b2, ai, tb)
            emit_w2(*prev)
            if f == 1 and l + 1 < L:
                cond_finish(l + 1)

        def load_w8(Win, pieces):
            b = nxt('w8', 2)
            fw.dma('pool', [lambda e, b=b, d=d, s=s, n=n: e.dma_start(out=W8[b][:, :, d:d + n], in_=Win[:, :, s:s + n])
                            for (d, s, n) in pieces], writes=[('W8', b)])
            return b

        def proj_chunk(b, wcol, tb):
            ps = psn()
            fw.ops('pe', [mm(PS[ps][:, :], lhsT=W8[b][:, k, wcol:wcol + 128], rhs=H[:, k, tbs(tb)],
                             start=(k == 0), stop=(k == 7)) for k in range(8)],
                   reads=[('W8', b)] + [('H', k, tb) for k in range(8)], writes=[('PS', ps)])
            return ps

        def proj_tm(Win, vcol, gcol, ng, pre=None):
            pieces = []
            nv = 0
            if vcol is not None:
                pieces.append((0, vcol, 256))
                nv = 256
            if ng:
                pieces.append((nv, gcol, ng))
            b = load_w8(Win, pieces)
            n = nv + ng
            for i in range(16):
                for fn_ in (pre or {}).get(i, ()):
                    fn_()
                ps = psn()
                fw.ops('pe', [mm(PS[ps][:, 0:n], lhsT=H[:, k, i * 128:(i + 1) * 128], rhs=W8[b][:, k, 0:n],
                                 start=(k == 0), stop=(k == 7)) for k in range(8)],
                       reads=[('W8', b)] + [('H', k, i // 4) for k in range(8)], writes=[('PS', ps)])
                if nv:
                    for u in range(2):
                        fw.op('act', lambda a, ps=ps, i=i, u=u: a.activation(
                            out=v_pair(i, u),
                            in_=PS[ps][:, 128 * u:128 * u + 128].rearrange("p (a b) -> p a b", b=64), func=AF.Copy),
                            reads=[('PS', ps)], writes=[('V', i)])
                if ng:
                    fw.op('act', lambda a, ps=ps, i=i: a.activation(out=GPT[:, i, 0:ng], in_=PS[ps][:, nv:nv + ng], func=AF.Copy),
                          reads=[('PS', ps)], writes=['GPT'])

        def bcast4(c0):
            return PP[:, c0:c0 + 4].unsqueeze(1).to_broadcast([128, 16, 4])

        def softplus_parts(z, tmp_a, out_l):
            fw.op('act', lambda a: a.activation(out=G[tmp_a][:], in_=G[z][:], func=AF.Abs),
                  reads=[('G', z)], writes=[('G', tmp_a)])
            fw.op('act', lambda a: a.activation(out=G[tmp_a][:], in_=G[tmp_a][:], func=AF.Exp, scale=-1.0),
                  reads=[('G', tmp_a)], writes=[('G', tmp_a)])
            fw.op('act', lambda a: a.activation(out=G[out_l][:], in_=G[tmp_a][:], func=AF.Ln, bias=ONE1[:, 0:1], scale=1.0),
                  reads=[('G', tmp_a), 'ONE1'], writes=[('G', out_l)])

        ONE1 = fw.sb("ONE1", [128, 1], F32)
        fw.op('pool', lambda g: g.memset(ONE1[:], 1.0), writes=['ONE1'])

        def cumsum(src, dst):
            p1 = psn()
            p2 = psn()
            flat = G[src][:].rearrange("p a b -> p (a b)")
            fw.ops('pe', [mm(PS[p1][:, 0:64], lhsT=TRI, rhs=flat, start=True, stop=True)],
                   reads=[('G', src), 'CF'], writes=[('PS', p1)])
            fw.ops('pe', [mm(PS[p2][:, 0:64], lhsT=ONESF, rhs=flat, start=True, stop=True)],
                   reads=[('G', src), 'CF'], writes=[('PS', p2)])
            tot = PS[p2][:, 0:64].rearrange("p (a b) -> p a b", b=4)
            fw.op('dve', lambda v: v.memset(PRE[:, 0, :], 0.0), writes=['PRE'])
            for i in range(1, 16):
                fw.op('dve', lambda v, i=i: v.tensor_tensor(out=PRE[:, i, :], in0=PRE[:, i - 1, :], in1=tot[:, i - 1, :],
                                                           op=ALU.add),
                      reads=['PRE', ('PS', p2)], writes=['PRE'])
            fw.op('dve', lambda v: v.tensor_tensor(out=G[dst][:], in0=PS[p1][:, 0:64].rearrange("p (a b) -> p a b", b=4),
                                                  in1=PRE[:], op=ALU.add),
                  reads=['PRE', ('PS', p1)], writes=[('G', dst)])

        SB3 = [fw.sb("SB3_%d" % i, [128, 16, 4], BF16) for i in range(3)]

        def build_aug(val, t1, t2):
            spl_keys = [('SPL', i) for i in range(16)]
            fw.op('dve', lambda v: v.tensor_copy(out=SB3[0][:], in_=G[val][:]), reads=[('G', val)], writes=[('SB3', 0)])
            fw.op('dve', lambda v: v.tensor_tensor(out=G[t1][:], in0=G[val][:], in1=SB3[0][:], op=ALU.subtract),
                  reads=[('G', val), ('SB3', 0)], writes=[('G', t1)])
            fw.op('dve', lambda v: v.tensor_copy(out=SB3[1][:], in_=G[t1][:]), reads=[('G', t1)], writes=[('SB3', 1)])
            fw.op('dve', lambda v: v.tensor_tensor(out=G[t2][:], in0=G[t1][:], in1=SB3[1][:], op=ALU.subtract),
                  reads=[('G', t1), ('SB3', 1)], writes=[('G', t2)])
            fw.op('dve', lambda v: v.tensor_copy(out=SB3[2][:], in_=G[t2][:]), reads=[('G', t2)], writes=[('SB3', 2)])
            v4 = SPL[:].rearrange("p a (h r) -> p a h r", r=64)
            for j in range(2):
                for r in range(3):
                    fw.op('dve', lambda v, j=j, r=r: v.tensor_copy(out=v4[:, :, :, r], in_=SB3[r][:, :, 2 * j:2 * j + 2]),
                          reads=[('SB3', r)], writes=spl_keys)
                for tb in range(4):
                    half = tb % 2
                    fw.ops('pe', [lambda t, i=i, half=half: t.transpose(
                        out=PSB[:, half * 512 + (i % 4) * 128:half * 512 + (i % 4 + 1) * 128], in_=SPL[:, i, :], identity=IDENT)
                        for i in range(4 * tb, 4 * tb + 4)],
                        reads=spl_keys + ['CB'], writes=[('PS', 7)])
                    fw.op('act', lambda a, tb=tb, half=half, j=j: a.activation(out=AUG[:, j, tbs(tb)],
                                                                               in_=PSB[:, half * 512:(half + 1) * 512],
                                                                               func=AF.Copy),
                          reads=[('PS', 7)], writes=[('AUG', j, tb)])

        def attention(mode, qk, vfn, M, post, pre_tb=None, share_qk=False):
            LOOK = 2
            if share_qk:
                tasks = [(tb, 2 * g + u, sc) for tb in range(4) for g in range(2) for sc in range(4 * tb + 4)
                         for u in range(2)]
            else:
                tasks = [(tb, h, sc) for tb in range(4) for h in range(4) for sc in range(4 * tb + 4)]
            acc = {}
            shared = {}

            def phase_a(tb, h, sc):
                qa, ka = qk(h)
                pq = 64 * (h % 2)
                slab = h // 2
                d = sc - 4 * tb
                scs = slice(sc * 128, (sc + 1) * 128)
                trow = lambda ps, first, last: mm(PS[ps][:, :], lhsT=CB[pq:pq + 64, CB_ONES:CB_ONES + 128],
                                                  rhs=AUG[pq:pq + 64, slab, tbs(tb)], start=first, stop=last)

                def maskmms(ps):
                    c1 = CB_MASK + 448 - 128 * d
                    c2 = c1 - 64
                    return [mm(PS[ps][:, :], lhsT=CB[pq:pq + 64, CB_SEL1:CB_SEL1 + 128], rhs=CB[pq:pq + 64, c1:c1 + 512],
                               start=False, stop=False),
                            mm(PS[ps][:, :], lhsT=CB[pq:pq + 64, CB_SEL2:CB_SEL2 + 128], rhs=CB[pq:pq + 64, c2:c2 + 512],
                               start=False, stop=True)]
                rd_qk = [qk_key(h, 'q', tb), qk_key(h, 'k', sc // 4)]
                rd_aug = [('AUG', slab, tb)]
                sbias = G[5][:, sc, h:h + 1]
                pt = ptn()
                if mode == 'softmax':
                    ps = psn()
                    fns = [mm(PS[ps][:, :], lhsT=ka[:, scs], rhs=qa[:, tbs(tb)], start=True, stop=False),
                           trow(ps, False, d < 0)]
                    if d >= 0:
                        fns += maskmms(ps)
                    fw.ops('pe', fns, reads=rd_qk + rd_aug + ['CB'], writes=[('PS', ps)])
                    fw.op('act', lambda a, ps=ps, pt=pt: a.activation(out=PTB[pt][:], in_=PS[ps][:], func=AF.Exp,
                                                                      bias=sbias, scale=1.0),
                          reads=[('PS', ps), ('G', 5)], writes=[('PT', pt)])
                else:
                    if share_qk and h % 2 == 1:
                        ps = shared['ps']
                    else:
                        ps = psn()
                        shared['ps'] = ps
                        fw.ops('pe', [mm(PS[ps][:, :], lhsT=ka[:, scs], rhs=qa[:, tbs(tb)], start=True, stop=True)],
                               reads=rd_qk, writes=[('PS', ps)])
                    pl = psn()
                    fns = [trow(pl, True, d < 0)]
                    if d >= 0:
                        fns += maskmms(pl)
                    fw.ops('pe', fns, reads=rd_aug + ['CB'], writes=[('PS', pl)])
                    e = tmpf()
                    fw.op('act', lambda a, pl=pl, e=e: a.activation(out=T[e][:], in_=PS[pl][:], func=AF.Exp,
                                                                    bias=sbias, scale=1.0),
                          reads=[('PS', pl), ('G', 5)], writes=[('T', e)])
                    fw.op('dve', lambda v, ps=ps, e=e, pt=pt: v.tensor_tensor(out=PTB[pt][:], in0=PS[ps][:],
                                                                            in1=T[e][:], op=ALU.mult),
                          reads=[('PS', ps), ('T', e)], writes=[('PT', pt)])
                return pt

            def phase_b(tb, h, sc, pt):
                nsc = 4 * tb + 4
                if sc == 0:
                    acc[h] = pon()
                po = acc[h]
                fw.ops('pe', [mm(PS[po][:, :], lhsT=vfn(h, sc), rhs=PTB[pt][:], start=(sc == 0), stop=(sc == nsc - 1))],
                       reads=[('PT', pt), ('V', sc)], writes=[('PS', po)])
                if sc == nsc - 1:
                    cont = post(h, tb, po)
                    if cont is not None:
                        deferred.append(cont)

            pend = []
            deferred = []
            for i0 in range(0, len(tasks), LOOK):
                for (tb, h, sc) in tasks[i0:i0 + LOOK]:
                    pend.append((tb, h, sc, phase_a(tb, h, sc)))
                while deferred:
                    deferred.pop(0)()
                while len(pend) > LOOK:
                    phase_b(*pend.pop(0))
            while pend:
                phase_b(*pend.pop(0))
                while deferred:
                    deferred.pop(0)()

        def qk_key(h, which, blk):
            return ('QK', h, which, blk)

        def head_norm_gate(l, h, tb, tsrc, gcol):
            pb = 64 * (h % 2)
            c = h // 2
            fw.op('act', lambda a: a.activation(out=A[2][pb:pb + 64, c, tbs(tb)], in_=T[tsrc][pb:pb + 64, :], func=AF.Square),
                  reads=[('T', tsrc)], writes=[('A', 2, c, tb)])

            def cont():
                head_norm_gate2(l, h, tb, tsrc, gcol)
            return cont

        def head_norm_gate2(l, h, tb, tsrc, gcol):
            pb = 64 * (h % 2)
            c = h // 2
            pn = psn()
            fw.ops('pe', [mm(PS[pn][0:64, :], lhsT=CB[pb:pb + 64, CB_ONES:CB_ONES + 64], rhs=A[2][pb:pb + 64, c, tbs(tb)],
                             start=True, stop=True)], reads=[('A', 2, c, tb), 'CB'], writes=[('PS', pn)])
            t2 = tmpf()
            fw.op('act', lambda a: a.activation(out=T[t2][pb:pb + 64, :], in_=PS[pn][0:64, :], func=AF.Sqrt,
                                                scale=1.0 / 64.0, bias=EPSB[pb:pb + 64, 0:1]),
                  reads=[('PS', pn), 'EPSB'], writes=[('T', t2)])
            fw.op('dve', lambda v: v.reciprocal(out=T[t2][pb:pb + 64, :], in_=T[t2][pb:pb + 64, :]),
                  reads=[('T', t2)], writes=[('T', t2)])
            fw.op('dve', lambda v: v.scalar_tensor_tensor(out=T[tsrc][pb:pb + 64, :], in0=T[tsrc][pb:pb + 64, :],
                                                         scalar=PP[pb:pb + 64, gcol + c:gcol + c + 1],
                                                         in1=T[t2][pb:pb + 64, :], op0=ALU.mult, op1=ALU.mult),
                  reads=[('T', tsrc), ('T', t2), 'PP'], writes=[('T', tsrc)])
            fw.op('dve', lambda v: v.tensor_tensor(out=A[2][pb:pb + 64, c, tbs(tb)], in0=T[tsrc][pb:pb + 64, :],
                                                  in1=A[3][pb:pb + 64, c, tbs(tb)], op=ALU.mult),
                  reads=[('T', tsrc), ('A', 3, c, tb)], writes=[('A', 2, c, tb)])

        def conv_silu(l, ps, wcol, bcol, cc, tb, out_ap, wkeys):
            if tb == 0:
                fw.op('dve', lambda v: v.memset(CS[:, 0:3], 0.0), writes=['CS'])
            fw.op('act', lambda a: a.activation(out=CS[:, 3:515], in_=PS[ps][:], func=AF.Copy),
                  reads=[('PS', ps)], writes=['CS'])
            t = tmpf()
            fw.op('dve', lambda v: v.tensor_scalar(out=T[t][:], in0=CS[:, 0:512], scalar1=PP[:, wcol + cc * 4:wcol + cc * 4 + 1],
                                                  scalar2=None, op0=ALU.mult), reads=['CS', 'PP'], writes=[('T', t)])
            for j in range(1, 4):
                fw.op('dve', lambda v, j=j: v.scalar_tensor_tensor(out=T[t][:], in0=CS[:, j:j + 512],
                                                                  scalar=PP[:, wcol + cc * 4 + j:wcol + cc * 4 + j + 1],
                                                                  in1=T[t][:], op0=ALU.mult, op1=ALU.add),
                      reads=['CS', 'PP', ('T', t)], writes=[('T', t)])
            if tb < 3:
                fw.op('dve', lambda v: v.tensor_copy(out=CS[:, 0:3], in_=CS[:, 512:515]), reads=['CS'], writes=['CS'])
            fw.op('act', lambda a: a.activation(out=out_ap, in_=T[t][:], func=AF.Silu, bias=PP[:, bcol + cc:bcol + cc + 1],
                                                scale=1.0), reads=[('T', t), 'PP'], writes=wkeys)

        def out_proj(l, mi):
            Wout = w_out[l].rearrange("(k p) n -> p k n", p=128)
            b2 = nxt('w2', 2)
            fw.dma('pool', lambda e: e.dma_start(out=W2[b2][:], in_=Wout[:, 2 * mi:2 * mi + 2, :]), writes=[('W2', b2)])
            for tb in range(4):
                emit_w2(b2, 2, tb)

        def std_qk(h):
            return A[0][64 * (h % 2):64 * (h % 2) + 64, h // 2, :], A[1][64 * (h % 2):64 * (h % 2) + 64, h // 2, :]

        def qk_writes(which, c, tb):
            return [qk_key(2 * c, which, tb), qk_key(2 * c + 1, which, tb), ('A', 0 if which == 'q' else 1, c, tb)]

        def v_pair(i, u):
            base = V[:, i, 192 * u:192 * u + 64]
            return bass.AP(tensor=base.tensor, offset=base.offset, ap=[list(base.ap[0]), [128, 2], [1, 64]])

        VAUG0 = [0, 64, 192, 256]
        VCOL = [0, 128, 192, 320]

        def v_aug(h, sc):
            return V[:, sc, VAUG0[h]:VAUG0[h] + 128]

        def v_plain(h, sc):
            return V[:, sc, VCOL[h]:VCOL[h] + 64]

        def mixer(l):
            o = PPL['L'][l]
            Win = w_in[l].rearrange("(k p) n -> p k n", p=128)
            set_gate(l, 5, 1.0)
            proj_tm(Win, 512, 768, 4, pre={0: [lambda: norm_tb(l, 1, 0), lambda: norm_tb(l, 1, 1)],
                                            4: [lambda: norm_tb(l, 1, 2)], 8: [lambda: norm_tb(l, 1, 3)]})
            if stop == 'fox_g1':
                return
            fw.op('dve', lambda v: v.tensor_tensor(out=G[0][:], in0=GPT[:, :, 0:4], in1=bcast4(o['ffb']), op=ALU.add),
                  reads=['GPT', 'PP'], writes=[('G', 0)])
            softplus_parts(0, 1, 2)
            fw.op('dve', lambda v: v.scalar_tensor_tensor(out=G[3][:], in0=G[0][:], scalar=0.0, in1=G[2][:],
                                                         op0=ALU.min, op1=ALU.subtract),
                  reads=[('G', 0), ('G', 2)], writes=[('G', 3)])
            if stop == 'fox_g2':
                return
            cumsum(3, 4)
            fw.op('dve', lambda v: v.tensor_scalar(out=G[5][:], in0=G[4][:], scalar1=-1.0, scalar2=None, op0=ALU.mult),
                  reads=[('G', 4)], writes=[('G', 5)])
            if stop == 'fox_g':
                return
            build_aug(4, 0, 1)
            if stop == 'fox_a':
                return
            b = load_w8(Win, [(0, 0, 512)])
            for cc in range(4):
                for tb in range(4):
                    ps = proj_chunk(b, cc * 128, tb)
                    if cc < 2:
                        fw.op('act', lambda a, ps=ps, cc=cc, tb=tb: a.activation(out=A[0][:, cc, tbs(tb)], in_=PS[ps][:],
                                                                                func=AF.Copy, scale=0.125),
                              reads=[('PS', ps)], writes=qk_writes('q', cc, tb))
                    else:
                        fw.op('act', lambda a, ps=ps, cc=cc, tb=tb: a.activation(out=A[1][:, cc - 2, tbs(tb)], in_=PS[ps][:],
                                                                                func=AF.Copy),
                              reads=[('PS', ps)], writes=qk_writes('k', cc - 2, tb))

            def post_fox(h, tb, po):
                pb = 64 * (h % 2)
                r = tmpf()
                fw.op('dve', lambda v: v.reciprocal(out=T[r][pb:pb + 64, :], in_=PS[po][64 - pb:128 - pb, :]),
                      reads=[('PS', po)], writes=[('T', r)])
                fw.op('dve', lambda v: v.tensor_tensor(out=A[2][pb:pb + 64, h // 2, tbs(tb)], in0=PS[po][pb:pb + 64, :],
                                                      in1=T[r][pb:pb + 64, :], op=ALU.mult),
                      reads=[('PS', po), ('T', r)], writes=[('A', 2, h // 2, tb)])
            attention('softmax', std_qk, v_aug, 128, post_fox)
            out_proj(l, 0)
            if stop == 'fox':
                return
            proj_tm(Win, 1284, 1540, 8)
            fw.op('dve', lambda v: v.tensor_tensor(out=G[6][:], in0=GPT[:, :, 0:4], in1=bcast4(o['mib']), op=ALU.add),
                  reads=['GPT', 'PP'], writes=[('G', 6)])
            fw.op('dve', lambda v: v.tensor_tensor(out=G[0][:], in0=GPT[:, :, 4:8], in1=bcast4(o['mfb']), op=ALU.add),
                  reads=['GPT', 'PP'], writes=[('G', 0)])
            softplus_parts(0, 1, 2)
            fw.op('dve', lambda v: v.scalar_tensor_tensor(out=G[3][:], in0=G[0][:], scalar=0.0, in1=G[2][:],
                                                         op0=ALU.min, op1=ALU.subtract),
                  reads=[('G', 0), ('G', 2)], writes=[('G', 3)])
            cumsum(3, 4)
            fw.op('dve', lambda v: v.scalar_tensor_tensor(out=G[5][:], in0=G[6][:], scalar=math.log(0.125), in1=G[4][:],
                                                         op0=ALU.add, op1=ALU.subtract),
                  reads=[('G', 6), ('G', 4)], writes=[('G', 5)])
            build_aug(4, 0, 1)
            b = load_w8(Win, [(0, 1548, 256)])
            for cc in range(2):
                for tb in range(4):
                    ps = proj_chunk(b, cc * 128, tb)
                    fw.op('act', lambda a, ps=ps, cc=cc, tb=tb: a.activation(out=A[3][:, cc, tbs(tb)], in_=PS[ps][:],
                                                                            func=AF.Sigmoid),
                          reads=[('PS', ps)], writes=[('A', 3, cc, tb)])
            b = load_w8(Win, [(0, 772, 512)])
            for cc in range(4):
                for tb in range(4):
                    ps = proj_chunk(b, cc * 128, tb)
                    if cc < 2:
                        conv_silu(l, ps, o['mcw'], o['mcb'], cc, tb, A[0][:, cc, tbs(tb)], qk_writes('q', cc, tb))
                    else:
                        conv_silu(l, ps, o['mcw'], o['mcb'], cc, tb, A[1][:, cc - 2, tbs(tb)], qk_writes('k', cc - 2, tb))

            def post_mlstm(h, tb, po):
                pb = 64 * (h % 2)
                r = tmpf()
                fw.op('act', lambda a: a.activation(out=T[r][pb:pb + 64, :], in_=PS[po][64 - pb:128 - pb, :], func=AF.Abs),
                      reads=[('PS', po)], writes=[('T', r)])
                fw.op('dve', lambda v: v.tensor_scalar(out=T[r][pb:pb + 64, :], in0=T[r][pb:pb + 64, :], scalar1=1.0,
                                                      scalar2=None, op0=ALU.max),
                      reads=[('T', r)], writes=[('T', r)])
                fw.op('dve', lambda v: v.reciprocal(out=T[r][pb:pb + 64, :], in_=T[r][pb:pb + 64, :]),
                      reads=[('T', r)], writes=[('T', r)])
                t = tmpf()
                fw.op('dve', lambda v: v.tensor_tensor(out=T[t][pb:pb + 64, :], in0=PS[po][pb:pb + 64, :],
                                                      in1=T[r][pb:pb + 64, :], op=ALU.mult),
                      reads=[('PS', po), ('T', r)], writes=[('T', t)])
                return head_norm_gate(l, h, tb, t, o['mng'])
            attention('linear', std_qk, v_aug, 128, post_mlstm)
            out_proj(l, 1)
            if stop == 'mlstm':
                return
            proj_tm(Win, 2316, 0, 0)
            fw.dma('sp', lambda e: e.dma_start(out=AUG[:], in_=rad),
                   writes=[('AUG', s_, t_) for s_ in range(2) for t_ in range(4)])
            fw.op('dve', lambda v: v.tensor_copy(out=G[5][:].rearrange("p a b -> p (a b)"), in_=CF[:, CF_RS:CF_RS + 64]),
                  reads=['CF'], writes=[('G', 5)])
            b = load_w8(Win, [(0, 2572, 256)])
            for cc in range(2):
                for tb in range(4):
                    ps = proj_chunk(b, cc * 128, tb)
                    fw.op('act', lambda a, ps=ps, cc=cc, tb=tb: a.activation(out=A[3][:, cc, tbs(tb)], in_=PS[ps][:],
                                                                            func=AF.Silu),
                          reads=[('PS', ps)], writes=[('A', 3, cc, tb)])
            for which, base in (('q', 1804), ('k', 2060)):
                pieces = [(0, base, 256)]
                for hh in range(4):
                    pieces.append((256 + 64 * hh, base + 64 * hh + 32, 32))
                    pieces.append((256 + 64 * hh + 32, base + 64 * hh, 32))
                b = load_w8(Win, pieces)
                dst = A[0] if which == 'q' else A[1]
                for tb in range(4):
                    fw.dma('sp', [lambda e, tb=tb: e.dma_start(out=ROPE[:, 0, :], in_=cfd[:, CF_COS + tb * 512:CF_COS + (tb + 1) * 512]),
                                  lambda e, tb=tb: e.dma_start(out=ROPE[:, 1, :], in_=cfd[:, CF_SIN + tb * 512:CF_SIN + (tb + 1) * 512])],
                           writes=['ROPE'])
                    for cc in range(2):
                        p1 = proj_chunk(b, cc * 128, tb)
                        p2 = proj_chunk(b, 256 + cc * 128, tb)
                        t1 = tmpf()
                        t2 = tmpf()
                        fw.op('dve', lambda v, p1=p1, t1=t1: v.tensor_tensor(out=T[t1][:], in0=PS[p1][:], in1=ROPE[:, 0, :],
                                                                             op=ALU.mult),
                              reads=[('PS', p1), 'ROPE'], writes=[('T', t1)])
                        fw.op('dve', lambda v, p2=p2, t2=t2: v.tensor_tensor(out=T[t2][:], in0=PS[p2][:], in1=ROPE[:, 1, :],
                                                                             op=ALU.mult),
                              reads=[('PS', p2), 'ROPE'], writes=[('T', t2)])
                        fw.op('dve', lambda v, t1=t1, t2=t2, cc=cc, tb=tb, dst=dst: v.tensor_tensor(
                            out=dst[:, cc, tbs(tb)], in0=T[t1][:], in1=T[t2][:], op=ALU.add),
                            reads=[('T', t1), ('T', t2)], writes=qk_writes(which, cc, tb))

            def post_ret(h, tb, po):
                pb = 64 * (h % 2)
                t = tmpf()
                fw.op('act', lambda a: a.activation(out=T[t][pb:pb + 64, :], in_=PS[po][pb:pb + 64, :], func=AF.Copy),
                      reads=[('PS', po)], writes=[('T', t)])
                return head_norm_gate(l, h, tb, t, o['rng'])
            attention('linear', std_qk, v_aug, 128, post_ret)
            out_proj(l, 2)
            if stop == 'ret':
                return
            proj_tm(Win, None, 3596, 4)
            fw.op('dve', lambda v: v.tensor_tensor(out=G[0][:], in0=GPT[:, :, 0:4], in1=bcast4(o['sdtb']), op=ALU.add),
                  reads=['GPT', 'PP'], writes=[('G', 0)])
            softplus_parts(0, 1, 2)
            fw.op('dve', lambda v: v.scalar_tensor_tensor(out=G[3][:], in0=G[0][:], scalar=0.0, in1=G[2][:],
                                                         op0=ALU.max, op1=ALU.add),
                  reads=[('G', 0), ('G', 2)], writes=[('G', 3)])
            fw.op('act', lambda a: a.activation(out=AEXP[:], in_=PP[:, o['sAl']:o['sAl'] + 4], func=AF.Exp),
                  reads=['PP'], writes=['AEXP'])
            fw.op('dve', lambda v: v.scalar_tensor_tensor(out=G[6][:], in0=G[3][:], scalar=-1.0,
                                                         in1=AEXP[:].unsqueeze(1).to_broadcast([128, 16, 4]),
                                                         op0=ALU.mult, op1=ALU.mult),
                  reads=[('G', 3), 'AEXP'], writes=[('G', 6)])
            cumsum(6, 4)
            fw.op('act', lambda a: a.activation(out=G[2][:], in_=G[3][:], func=AF.Ln), reads=[('G', 3)], writes=[('G', 2)])
            fw.op('dve', lambda v: v.tensor_tensor(out=G[5][:], in0=G[2][:], in1=G[4][:], op=ALU.subtract),
                  reads=[('G', 2), ('G', 4)], writes=[('G', 5)])
            build_aug(4, 0, 1)
            if stop == 'fox_ssd_g':
                return
            b = load_w8(Win, [(0, 2828, 256)])
            for cc in range(2):
                for tb in range(4):
                    ps = proj_chunk(b, cc * 128, tb)
                    fw.op('act', lambda a, ps=ps, cc=cc, tb=tb: a.activation(out=A[3][:, cc, tbs(tb)], in_=PS[ps][:],
                                                                            func=AF.Silu),
                          reads=[('PS', ps)], writes=[('A', 3, cc, tb)])
            b = load_w8(Win, [(0, 3084, 512)])
            dsts = [(0, 1), (1, 1), (1, 0), (0, 0)]
            for cc in range(4):
                ai, ac = dsts[cc]
                for tb in range(4):
                    ps = proj_chunk(b, cc * 128, tb)
                    wk = [('A', ai, ac, tb)]
                    if cc == 2:
                        wk += [qk_key(hh, 'k', tb) for hh in range(4)]
                    if cc == 3:
                        wk += [qk_key(hh, 'q', tb) for hh in range(4)]
                    conv_silu(l, ps, o['scw'], o['scb'], cc, tb, A[ai][:, ac, tbs(tb)], wk)
            for i in range(16):
                half = i % 2
                fw.ops('pe', [lambda t, i=i, c=c, half=half: t.transpose(
                    out=PSB[:, half * 512 + c * 128:half * 512 + (c + 1) * 128],
                    in_=A[c][:, 1, i * 128:(i + 1) * 128], identity=IDENT) for c in range(2)],
                    reads=[('A', 0, 1, i // 4), ('A', 1, 1, i // 4), 'CB'], writes=[('PS', 7)])
                for u in range(2):
                    fw.op('act', lambda a, i=i, half=half, u=u: a.activation(
                        out=v_pair(i, u),
                        in_=PSB[:, half * 512 + 128 * u:half * 512 + 128 * u + 128].rearrange("p (a b) -> p a b", b=64),
                        func=AF.Copy), reads=[('PS', 7)], writes=[('V', i)])

            if stop == 'fox_ssd_p':
                return

            def ssd_qk(h):
                g = h // 2
                return A[0][64 * g:64 * g + 64, 0, :], A[1][64 * g:64 * g + 64, 0, :]
            ssd_tiles = {}

            def pre_ssd(tb):
                pass

            def post_ssd(h, tb, po):
                if os.environ.get('SKIP_SSD_POST') == '1':
                    return
                pb = 64 * (h % 2)
                c = h // 2
                xh = A[c][pb:pb + 64, 1, tbs(tb)]
                fw.op('dve', lambda v: v.scalar_tensor_tensor(out=ROPE[pb:pb + 64, c, :], in0=xh,
                                                             scalar=PP[pb:pb + 64, o['sD'] + c:o['sD'] + c + 1],
                                                             in1=PS[po][pb:pb + 64, :], op0=ALU.mult, op1=ALU.add),
                      reads=[('A', c, 1, tb), 'PP', ('PS', po)], writes=['ROPE'])
                fw.op('dve', lambda v: v.tensor_tensor(out=ROPE[pb:pb + 64, c, :], in0=ROPE[pb:pb + 64, c, :],
                                                      in1=A[3][pb:pb + 64, c, tbs(tb)], op=ALU.mult),
                      reads=['ROPE', ('A', 3, c, tb)], writes=['ROPE'])
                if h == 3 and os.environ.get('SKIP_SSD_POST') != '2':
                    y0, y1 = 0, 1
                    ykeys = ['ROPE']
                    for c2, yy in enumerate((y0, y1)):
                        fw.op('act', lambda a, yy=yy, c2=c2: a.activation(out=A[2][:, c2, tbs(tb)], in_=ROPE[:, yy, :],
                                                                          func=AF.Square),
                              reads=ykeys, writes=[('A', 2, c2, tb)])
                    return lambda: post_ssd2(tb, ykeys, y0, y1)

            def post_ssd2(tb, ykeys, y0, y1):
                if True:
                    pn = psn()
                    fw.ops('pe', [mm(PS[pn][:, :], lhsT=ONESB, rhs=A[2][:, c2, tbs(tb)], start=(c2 == 0), stop=(c2 == 1))
                                  for c2 in range(2)], reads=[('A', 2, 0, tb), ('A', 2, 1, tb), 'CB'], writes=[('PS', pn)])
                    r = tmpf()
                    fw.op('act', lambda a: a.activation(out=T[r][:], in_=PS[pn][:], func=AF.Sqrt, scale=1.0 / 256.0,
                                                        bias=EPSB[:, 0:1]), reads=[('PS', pn), 'EPSB'], writes=[('T', r)])
                    fw.op('dve', lambda v: v.reciprocal(out=T[r][:], in_=T[r][:]), reads=[('T', r)], writes=[('T', r)])
                    for c2, yy in enumerate((y0, y1)):
                        fw.op('dve', lambda v, c2=c2, yy=yy: v.scalar_tensor_tensor(
                            out=A[2][:, c2, tbs(tb)], in0=ROPE[:, yy, :], scalar=PP[:, o['sng'] + c2:o['sng'] + c2 + 1],
                            in1=T[r][:], op0=ALU.mult, op1=ALU.mult),
                            reads=ykeys + [('T', r), 'PP'], writes=[('A', 2, c2, tb)])
            attention('linear', ssd_qk, v_aug, 128, post_ssd, share_qk=True)
            out_proj(l, 3)

        AEXP = fw.sb("AEXP", [128, 4], F32)

        done = False
        for l in range(L):
            if stop == 'h0':
                norm_mod(l, 0)
                break
            norm_prep(l, 0)
            ffn(l, 0, pre={0: [lambda l=l: norm_tb(l, 0, 0), lambda l=l: norm_tb(l, 0, 1)],
                           1: [lambda l=l: norm_tb(l, 0, 2)], 2: [lambda l=l: norm_tb(l, 0, 3)]})
            if stop == 'ffn1':
                break
            norm_prep(l, 1)
            mixer(l)
            if stop is not None and (stop in ('fox', 'mlstm', 'ret', 'mix') or stop.startswith('fox_')):
                break
            norm_prep(l, 2)
            ffn(l, 1, pre={0: [lambda l=l: norm_tb(l, 2, 0), lambda l=l: norm_tb(l, 2, 1)],
                           1: [lambda l=l: norm_tb(l, 2, 2)], 2: [lambda l=l: norm_tb(l, 2, 3)]})
        out_toks = []
        if stop == 'h0':
            for c in range(8):
                for tb in range(4):
                    t = tmpf()
                    fw.op('dve', lambda v, c=c, tb=tb, t=t: v.tensor_copy(out=T[t][:], in_=H[:, c, tbs(tb)]),
                          reads=[('H', c, tb)], writes=[('T', t)])
                    out_toks.append(fw.dma('sp', lambda e, c=c, tb=tb, t=t: e.dma_start(
                        out=yT[c * 128:(c + 1) * 128, tbs(tb)], in_=T[t][:]), reads=[('T', t)]))
        elif stop is not None:
            for c in range(8):
                out_toks.append(fw.dma('sp', lambda e, c=c: e.dma_start(out=yT[c * 128:(c + 1) * 128, :], in_=X[:, c, :]),
                                       reads=[('X', c, tb) for tb in range(4)]))
        else:
            SQv = A[0][:].rearrange("p a (b n) -> p (a b) n", n=512)
            a0keys = [('A', 0, c, t) for c in range(2) for t in range(4)]
            for tb in range(4):
                fw.op('act', lambda a, tb=tb: a.activation(out=SQv, in_=X[:, :, tbs(tb)], func=AF.Square),
                      reads=[('X', c, tb) for c in range(8)], writes=a0keys)
                ps = psn()
                fw.ops('pe', [mm(PS[ps][:, :], lhsT=ONESB, rhs=SQv[:, c, :], start=(c == 0), stop=(c == 7))
                              for c in range(8)], reads=a0keys + ['CB'], writes=[('PS', ps)])
                fw.op('act', lambda a, ps=ps: a.activation(out=RSTD, in_=PS[ps][:], func=AF.Sqrt,
                                                           scale=1.0 / 1024.0, bias=EPSB[:, 0:1]),
                      reads=[('PS', ps), 'EPSB'], writes=['CS'])
                fw.op('dve', lambda v: v.reciprocal(out=RSTD, in_=RSTD), reads=['CS'], writes=['CS'])
                for c in range(8):
                    fw.op('dve', lambda v, c=c, tb=tb: v.scalar_tensor_tensor(
                        out=X[:, c, tbs(tb)], in0=X[:, c, tbs(tb)], scalar=PP[:, PPL['fg'] + c:PPL['fg'] + c + 1],
                        in1=RSTD, op0=ALU.mult, op1=ALU.mult),
                        reads=[('X', c, tb), 'CS', 'PP'], writes=[('X', c, tb)])
            for c in range(8):
                out_toks.append(fw.dma('sp', lambda e, c=c: e.dma_start(out=yT[c * 128:(c + 1) * 128, :], in_=X[:, c, :]),
                                       reads=[('X', c, tb) for tb in range(4)]))
        fw.wait_all('sp', out_toks)
        fw.emit()
        print("kernel build: instructions", fw.n_ins, "waits", fw.n_wait, {e: fw.cnt[e] for e in ENGS}, flush=True)
    return nc


_CONSTS = None


def make_in_maps(inp, cores, NLW=4):
    global _CONSTS
    if _CONSTS is None:
        _CONSTS = make_consts()
    cb, cf, ra = _CONSTS
    f32 = lambda a: np.ascontiguousarray(np.asarray(a, np.float32))
    shared = {k: f32(inp[k][:NLW]) for k in ('ada_w', 'ffn1_w13', 'ffn1_w2', 'ffn2_w13', 'ffn2_w2', 'w_in', 'w_out')}
    maps = []
    for b in cores:
        m = dict(shared)
        m['xT'] = np.ascontiguousarray(np.asarray(inp['x'][b], np.float32).T)
        m['pp'] = pack_pp(inp, b)
        m['cb'] = cb
        m['cf'] = cf
        m['retaug'] = ra
        maps.append(m)
    return maps


def kernel(**inputs):
    inp = {k: np.asarray(v) for k, v in inputs.items()}
    nc = build(L=4)
    maps = make_in_maps(inp, list(range(8)))
    res = run_bass_kernel_spmd(nc, maps, core_ids=list(range(8)))
    out = np.stack([np.ascontiguousarray(res.results[b]['yT'].T) for b in range(8)], axis=0)
    return out.astype(np.float32)
```

```python
import math
import os
import numpy as np
import ml_dtypes
from contextlib import ExitStack
import concourse.bass as bass
import concourse.mybir as mybir
from concourse.bass_utils import run_bass_kernel_spmd

F32 = mybir.dt.float32
BF16 = mybir.dt.bfloat16
AF = mybir.ActivationFunctionType
ALU = mybir.AluOpType

ENGS = ['pe', 'act', 'dve', 'pool', 'sp']
N_DMA_SEMS = 12
S = 2048
NTB = 4
EPS = 1e-6
NEG = -30000.0


class Fw:
    def __init__(self, nc, ctx, self_sync=True):
        self.nc = nc
        self.ctx = ctx
        self.self_sync = self_sync
        self.q = {e: [] for e in ENGS}
        self.cnt = {e: 0 for e in ENGS}
        self.semobj = {}
        for e in ['pe', 'act', 'dve', 'pool']:
            self.semobj['s_' + e] = ctx.enter_context(nc.semaphore('s_' + e))
        for p in 'dw':
            for i in range(N_DMA_SEMS):
                self.semobj['%s%d' % (p, i)] = ctx.enter_context(nc.semaphore('%s%d' % (p, i)))
        self.dcnt = {p: [0] * N_DMA_SEMS for p in 'dw'}
        self.dlast = {p: [None] * N_DMA_SEMS for p in 'dw'}
        self.dnext = {p: 0 for p in 'dw'}
        self.last_w = {}
        self.readers = {}
        self.seen = {e: {} for e in ENGS}
        self.n_wait = 0
        self.n_ins = 0

    def sb(self, name, shape, dt):
        return self.ctx.enter_context(self.nc.sbuf_tensor(name, shape, dt))

    def ps(self, name, shape, dt=F32):
        return self.ctx.enter_context(self.nc.psum_tensor(name, shape, dt))

    def _deps(self, e, reads, writes):
        toks = []
        for k in reads:
            t = self.last_w.get(k)
            if t is not None:
                toks.append(t)
            if isinstance(k, tuple) and k[0] == 'PS':
                toks.extend(self.readers.get(k, ()))
        for k in writes:
            t = self.last_w.get(k)
            if t is not None:
                toks.append(t)
            toks.extend(self.readers.get(k, ()))
        need = {}
        for (sk, val, te) in toks:
            if te == e and (e == 'pe' or not self.self_sync):
                continue
            if self.seen[e].get(sk, 0) >= val:
                continue
            if need.get(sk, 0) < val:
                need[sk] = val
        for sk, val in need.items():
            self.seen[e][sk] = val
        return list(need.items())

    def _commit(self, tok, reads, writes):
        for k in reads:
            lst = self.readers.setdefault(k, [])
            lst[:] = [t for t in lst if t[0] != tok[0]]
            lst.append(tok)
        for k in writes:
            self.last_w[k] = tok
            self.readers[k] = []

    def ops(self, e, fns, reads=(), writes=()):
        if not isinstance(fns, (list, tuple)):
            fns = [fns]
        waits = self._deps(e, reads, writes)
        self.cnt[e] += 1
        tok = ('s_' + e, self.cnt[e], e)
        n = len(fns)
        for i, fn in enumerate(fns):
            self.q[e].append((fn, waits if i == 0 else [], ('s_' + e, 1) if i == n - 1 else None))
        self._commit(tok, reads, writes)
        self.n_ins += n
        self.n_wait += len(waits)
        return tok

    op = ops

    def dma(self, e, fns, reads=(), writes=()):
        if not isinstance(fns, (list, tuple)):
            fns = [fns]
        p = 'w' if e == 'pool' else 'd'
        i = self.dnext[p]
        self.dnext[p] = (i + 1) % N_DMA_SEMS
        sk = '%s%d' % (p, i)
        waits = self._deps(e, reads, writes)
        prev = self.dlast[p][i]
        if prev is not None and self.seen[e].get(sk, 0) < prev[1]:
            waits.append((sk, prev[1]))
            self.seen[e][sk] = prev[1]
        self.dcnt[p][i] += 16 * len(fns)
        tok = (sk, self.dcnt[p][i], 'dma')
        self.dlast[p][i] = tok
        for j, fn in enumerate(fns):
            self.q[e].append((fn, waits if j == 0 else [], (sk, 16)))
        self._commit(tok, reads, writes)
        self.n_ins += len(fns)
        self.n_wait += len(waits)
        return tok

    def wait_all(self, e, toks):
        waits = []
        for (sk, val, te) in toks:
            if self.seen[e].get(sk, 0) < val:
                waits.append((sk, val))
                self.seen[e][sk] = val
        self.q[e].append((None, waits, None))

    def emit(self):
        nc = self.nc
        with nc.Block() as block:
            def run(e):
                def body(engine):
                    for (fn, waits, inc) in self.q[e]:
                        for (sk, val) in waits:
                            engine.wait_ge(self.semobj[sk], val)
                        if fn is None:
                            continue
                        ins = fn(engine)
                        if inc is not None:
                            ins.then_inc(self.semobj[inc[0]], inc[1])
                return body
            block.sync(run('sp'))
            block.scalar(run('act'))
            block.vector(run('dve'))
            block.gpsimd(run('pool'))
            block.tensor(run('pe'))


def _pp_layout():
    off = [0]

    def al(n):
        o = off[0]
        off[0] += n
        return o
    lay = {'c': al(8), 'fg': al(8), 'L': []}
    for l in range(4):
        lay['L'].append(dict(ada_b=al(72), ng=al(24), mcw=al(16), mcb=al(4), scw=al(16), scb=al(4),
                             mng=al(2), rng=al(2), sng=al(2), sD=al(2),
                             ffb=al(4), mib=al(4), mfb=al(4), sdtb=al(4), sAl=al(4)))
    lay['n'] = off[0]
    return lay


PPL = _pp_layout()
CB_ID, CB_MASK, CB_ONES, CB_SEL1, CB_SEL2, NCB = 0, 128, 128 + 960, 128 + 960 + 128, 128 + 960 + 256, 128 + 960 + 384
CF_TRI, CF_ONES, CF_RS, CF_COS, CF_SIN, NCF = 0, 128, 256, 320, 320 + 2048, 320 + 4096


def col(w, n):
    return np.ascontiguousarray(np.asarray(w, np.float32).reshape(n, 128).T)


def pack_pp(inp, b):
    pp = np.zeros((128, PPL['n']), np.float32)
    pp[:, PPL['c']:PPL['c'] + 8] = col(inp['c'][b], 8)
    pp[:, PPL['fg']:PPL['fg'] + 8] = col(inp['final_g'], 8)
    for l in range(4):
        o = PPL['L'][l]
        pp[:, o['ada_b']:o['ada_b'] + 72] = col(inp['ada_b'][l], 72)
        for i in range(3):
            pp[:, o['ng'] + 8 * i:o['ng'] + 8 * i + 8] = col(inp['norm_g'][l, i], 8)
        for j in range(4):
            pp[:, o['mcw'] + j:o['mcw'] + 16:4] = col(inp['mlstm_conv_w'][l, j], 4)
            pp[:, o['scw'] + j:o['scw'] + 16:4] = col(inp['ssd_conv_w'][l, j], 4)
        pp[:, o['mcb']:o['mcb'] + 4] = col(inp['mlstm_conv_b'][l], 4)
        pp[:, o['scb']:o['scb'] + 4] = col(inp['ssd_conv_b'][l], 4)
        pp[:, o['mng']:o['mng'] + 2] = col(inp['mlstm_norm_g'][l], 2)
        pp[:, o['rng']:o['rng'] + 2] = col(inp['ret_norm_g'][l], 2)
        pp[:, o['sng']:o['sng'] + 2] = col(inp['ssd_norm_g'][l], 2)
        pp[:, o['sD']:o['sD'] + 2] = col(np.repeat(inp['ssd_D'][l], 64), 2)
        for nm, key in (('ffb', 'fox_fb'), ('mib', 'mlstm_ib'), ('mfb', 'mlstm_fb'), ('sdtb', 'ssd_dt_bias'),
                        ('sAl', 'ssd_A_log')):
            pp[:, o[nm]:o[nm] + 4] = np.broadcast_to(inp[key][l][None, :], (128, 4))
    return pp


def split3(x):
    x = np.asarray(x, np.float32)
    hi = x.astype(ml_dtypes.bfloat16)
    r = x - hi.astype(np.float32)
    mid = r.astype(ml_dtypes.bfloat16)
    r2 = r - mid.astype(np.float32)
    lo = r2.astype(ml_dtypes.bfloat16)
    return hi, mid, lo


def make_consts():
    cb = np.zeros((128, NCB), np.float32)
    cb[:, CB_ID:CB_ID + 128] = np.eye(128)
    kk = (np.arange(128) % 64)[:, None]
    uu = np.arange(960)[None, :]
    cb[:, CB_MASK:CB_MASK + 960] = np.where(kk <= uu - 448, 0.0, NEG)
    for k in range(128):
        cb[k, CB_SEL1 + (k % 64)] = 1.0
        cb[k, CB_SEL2 + (k % 64) + 64] = 1.0
    cb[:, CB_ONES:CB_ONES + 128] = 1.0
    cb = cb.astype(ml_dtypes.bfloat16)
    cf = np.zeros((128, NCF), np.float32)
    cf[:, CF_TRI:CF_TRI + 128] = (np.arange(128)[:, None] <= np.arange(128)[None, :]).astype(np.float32)
    cf[:, CF_ONES:CF_ONES + 128] = 1.0
    d = 64
    inv = (1.0 / (np.float32(10000.0) ** (np.arange(0, d, 2, dtype=np.float32) / np.float32(d)))).astype(np.float32)
    ang = np.arange(S, dtype=np.float32)[:, None] * inv[None, :]
    cos, sin = np.cos(ang).astype(np.float32), np.sin(ang).astype(np.float32)
    for p in range(128):
        i = p % 32
        cf[p, CF_COS:CF_COS + S] = cos[:, i]
        cf[p, CF_SIN:CF_SIN + S] = (-sin[:, i]) if (p % 64) < 32 else sin[:, i]
    ra = np.zeros((128, 2, S), ml_dtypes.bfloat16)
    t = np.arange(S, dtype=np.float64)
    rs = np.zeros((128, 16, 4), np.float32)
    for h in range(4):
        lg = np.log(np.float32(1.0) - np.float32(2.0) ** np.float32(-5.0 - h)).astype(np.float32)
        T = (t * np.float64(lg)).astype(np.float32)
        Sv = (-(t * np.float64(lg)) + math.log(0.125)).astype(np.float32)
        for r, v in enumerate(split3(T)):
            ra[64 * (h % 2) + r, h // 2] = v
        rs[:, :, h] = Sv.reshape(16, 128).T
    cf[:, CF_RS:CF_RS + 64] = rs.reshape(128, 64)
    return cb, cf, ra


def mm(out, lhsT, rhs, start, stop, **kw):
    return lambda t: t.matmul(out, lhsT=lhsT, rhs=rhs, start=start, stop=stop, **kw)


def build(L=4, stop=None, self_sync=True, NLW=4):
    nc = bass.Bass("TRN2", target_bir_lowering=False)

    def D(name, shape, dt, kind="ExternalInput"):
        return nc.dram_tensor(name, shape, dt, kind=kind).ap()
    xT = D("xT", [1024, S], F32)
    ppd = D("pp", [128, PPL['n']], F32)
    cbd = D("cb", [128, NCB], BF16)
    cfd = D("cf", [128, NCF], F32)
    rad = D("retaug", [128, 2, S], BF16)
    ada_w = D("ada_w", [NLW, 1024, 9216], F32)
    w13d = [D("ffn1_w13", [NLW, 1024, 5632], F32), D("ffn2_w13", [NLW, 1024, 5632], F32)]
    w2d = [D("ffn1_w2", [NLW, 2816, 1024], F32), D("ffn2_w2", [NLW, 2816, 1024], F32)]
    w_in = D("w_in", [NLW, 1024, 3600], F32)
    w_out = D("w_out", [NLW, 1024, 1024], F32)
    yT = D("yT", [1024, S], F32, kind="ExternalOutput")

    with ExitStack() as ctx:
        fw = Fw(nc, ctx, self_sync=self_sync)
        X = fw.sb("X", [128, 8, S], F32)
        H = fw.sb("H", [128, 8, S], BF16)
        W8 = [fw.sb("W8_%d" % i, [128, 8, 512], BF16) for i in range(2)]
        W2 = [fw.sb("W2_%d" % i, [128, 2, 1024], BF16) for i in range(2)]
        A = [fw.sb("A_%d" % i, [128, 2, S], BF16) for i in range(4)]
        AUG = fw.sb("AUG", [128, 2, S], BF16)
        V = fw.sb("V", [128, 16, 384], BF16)
        SPL = fw.sb("SPL", [128, 16, 128], BF16)
        NT = 5
        T = [fw.sb("T_%d" % i, [128, 512], F32) for i in range(NT)]
        NPT = 4
        PTB = [fw.sb("PT_%d" % i, [128, 512], BF16) for i in range(NPT)]
        CS = fw.sb("CS", [128, 516], F32)
        RSTD = CS[:, 0:512]
        ROPE = fw.sb("ROPE", [128, 2, 512], F32)
        PP = fw.sb("PP", [128, PPL['n']], F32)
        CB = fw.sb("CB", [128, NCB], BF16)
        CF = fw.sb("CF", [128, 320], F32)
        COND = fw.sb("COND", [128, 4, 72], F32)
        CA2 = fw.sb("CA2", [128, 8, 2], F32)
        AB = fw.sb("AB", [128, 8], F32)
        GT8 = fw.sb("GT8", [128, 8], F32)
        GPT = fw.sb("GPT", [128, 16, 8], F32)
        G = [fw.sb("G_%d" % i, [128, 16, 4], F32) for i in range(7)]
        PRE = fw.sb("PRE", [128, 16, 4], F32)
        NPS = 7
        PS = [fw.ps("PS_%d" % i, [128, 512], F32) for i in range(NPS)]
        PSB = fw.ps("PSB", [128, 1024], BF16)
        PS.append(PSB[:].bitcast(F32))
        ROTB = [0, 1, 2, 3, 4, 7]
        IDENT = CB[:, CB_ID:CB_ID + 128]
        ONESB = CB[:, CB_ONES:CB_ONES + 128]
        TRI = CF[:, CF_TRI:CF_TRI + 128]
        ONESF = CF[:, CF_ONES:CF_ONES + 128]

        rot = {'ps': 0, 't': 0, 'pt': 0, 'w8': 0, 'w2': 0, 'hs': 0}

        def nxt(k, n):
            v = rot[k]
            rot[k] = (v + 1) % n
            return v

        rot['po'] = 0

        def psn():
            return ROTB[nxt('ps', len(ROTB))]

        def pon():
            return NPS - 2 + nxt('po', 2)

        def tmpf():
            return nxt('t', NT)

        def ptn():
            return nxt('pt', NPT)

        def tbs(tb):
            return slice(tb * 512, (tb + 1) * 512)

        fw.dma('sp', lambda e: e.dma_start(out=PP[:], in_=ppd), writes=['PP'])
        fw.dma('sp', lambda e: e.dma_start(out=CB[:], in_=cbd), writes=['CB'])
        fw.dma('sp', lambda e: e.dma_start(out=CF[:], in_=cfd[:, 0:320]), writes=['CF'])
        for c in range(8):
            fw.dma('act', lambda e, c=c: e.dma_start(out=X[:, c, :], in_=xT[c * 128:(c + 1) * 128, :]),
                   writes=[('X', c, tb) for tb in range(4)])
        fw.op('pool', lambda g: g.memset(V[:, :, 64:128], 1.0), writes=[('V', i) for i in range(16)])
        fw.op('pool', lambda g: g.memset(V[:, :, 256:320], 1.0), writes=[('V', i) for i in range(16)])
        print("sbuf bytes remaining", nc.sbuf_bytes_remaining, flush=True)
        fw.op('pool', lambda g: g.memset(SPL[:], 0.0), writes=[('SPL', i) for i in range(16)])
        fw.op('pool', lambda g: g.memset(CS[:], 0.0), writes=['CS'])
        for j in range(2):
            fw.op('act', lambda a, j=j: a.activation(out=CA2[:, :, j], in_=PP[:, PPL['c']:PPL['c'] + 8], func=AF.Silu),
                  reads=['PP'], writes=['CA2'])
        Hf = H[:].bitcast(F32)
        CPSI = NPS - 1
        gi = 0
        for l in range(1):
            awl = ada_w[l].rearrange("(k p) n -> p k n", p=128)
            for g in range(18):
                b = gi % 2
                gi += 1
                keys = [('H', k, 2 * b + u) for k in range(8) for u in range(2)]
                fw.dma('sp', lambda e, b=b, g=g, awl=awl: e.dma_start(out=Hf[:, :, b * 512:(b + 1) * 512],
                                                                     in_=awl[:, :, g * 512:(g + 1) * 512]),
                       writes=keys)
                fns = []
                for j in range(4):
                    n = g * 4 + j
                    for k in range(8):
                        fns.append(mm(PS[CPSI][:, 2 * n:2 * n + 2],
                                      lhsT=Hf[:, k, b * 512 + j * 128:b * 512 + (j + 1) * 128],
                                      rhs=CA2[:, k, :], start=(k == 0), stop=(k == 7)))
                fw.ops('pe', fns, reads=keys + ['CA2'], writes=[('PS', CPSI)])
            ab = PPL['L'][l]['ada_b']
            fw.op('dve', lambda v, l=l, ab=ab: v.tensor_tensor(out=COND[:, l, :], in0=PS[CPSI][:, 0:144:2],
                                                             in1=PP[:, ab:ab + 72], op=ALU.add),
                  reads=[('PS', CPSI), 'PP'], writes=[('COND', l)])

        def cond_piece(l, p):
            bi = 2 + p % 2
            Af8 = A[bi][:].bitcast(F32).rearrange("p a (k n) -> p (a k) n", n=256)
            keys = [('A', bi, c, t) for c in range(2) for t in range(4)]
            awl = ada_w[l].rearrange("(k p) n -> p k n", p=128)
            fw.dma('sp', lambda e: e.dma_start(out=Af8, in_=awl[:, :, p * 256:(p + 1) * 256]), writes=keys)
            fns = []
            for j in range(2):
                n = 2 * p + j
                for k in range(8):
                    fns.append(mm(PS[CPSI][:, 2 * n:2 * n + 2], lhsT=Af8[:, k, j * 128:(j + 1) * 128], rhs=CA2[:, k, :],
                                  start=(k == 0), stop=(k == 7)))
            fw.ops('pe', fns, reads=keys + ['CA2'], writes=[('PS', CPSI)])

        def cond_finish(l):
            ab = PPL['L'][l]['ada_b']
            fw.op('dve', lambda v: v.tensor_tensor(out=COND[:, l, :], in0=PS[CPSI][:, 0:144:2],
                                                  in1=PP[:, ab:ab + 72], op=ALU.add),
                  reads=[('PS', CPSI), 'PP'], writes=[('COND', l)])

        def norm_prep(l, i):
            o = PPL['L'][l]
            sc0 = (3 * i + 1) * 8
            fw.op('dve', lambda v: v.scalar_tensor_tensor(out=AB[:], in0=COND[:, l, sc0:sc0 + 8], scalar=1.0,
                                                         in1=PP[:, o['ng'] + 8 * i:o['ng'] + 8 * i + 8],
                                                         op0=ALU.add, op1=ALU.mult),
                  reads=[('COND', l), 'PP'], writes=['AB'])

        def norm_tb(l, i, tb):
            sh0 = (3 * i) * 8
            SQv = A[3][:].rearrange("p a (b n) -> p (a b) n", n=512)
            sqkeys = [('A', 3, c, t) for c in range(2) for t in range(4)]
            fw.op('act', lambda a: a.activation(out=SQv, in_=X[:, :, tbs(tb)], func=AF.Square),
                  reads=[('X', c, tb) for c in range(8)], writes=sqkeys)
            ps = psn()
            fw.ops('pe', [mm(PS[ps][:, :], lhsT=ONESB, rhs=SQv[:, c, :], start=(c == 0), stop=(c == 7))
                          for c in range(8)], reads=sqkeys + ['CB'], writes=[('PS', ps)])
            fw.op('act', lambda a: a.activation(out=RSTD, in_=PS[ps][:], func=AF.Sqrt,
                                                scale=1.0 / 1024.0, bias=EPSB[:, 0:1]),
                  reads=[('PS', ps), 'EPSB'], writes=['CS'])
            fw.op('dve', lambda v: v.reciprocal(out=RSTD, in_=RSTD), reads=['CS'], writes=['CS'])
            for c in range(8):
                t2 = tmpf()
                fw.op('dve', lambda v, c=c, t2=t2: v.tensor_tensor(out=T[t2][:], in0=X[:, c, tbs(tb)],
                                                                   in1=RSTD, op=ALU.mult),
                      reads=[('X', c, tb), 'CS'], writes=[('T', t2)])
                fw.op('act', lambda a, c=c, t2=t2: a.activation(out=H[:, c, tbs(tb)], in_=T[t2][:],
                                                                func=AF.Identity, scale=AB[:, c:c + 1],
                                                                bias=COND[:, l, sh0 + c:sh0 + c + 1]),
                      reads=[('T', t2), 'AB', ('COND', l)], writes=[('H', c, tb)])

        def norm_mod(l, i):
            norm_prep(l, i)
            for tb in range(4):
                norm_tb(l, i, tb)

        EPSB = fw.sb("EPSB", [128, 1], F32)
        fw.op('pool', lambda g: g.memset(EPSB[:], EPS), writes=['EPSB'])

        def emit_w2(b, ai, tb):
            for m in range(8):
                ps = psn()
                fw.ops('pe', [mm(PS[ps][:, :], lhsT=W2[b][:, c, m * 128:(m + 1) * 128], rhs=A[ai][:, c, tbs(tb)],
                                 start=(c == 0), stop=(c == 1)) for c in range(2)],
                       reads=[('W2', b), ('A', ai, 0, tb), ('A', ai, 1, tb)], writes=[('PS', ps)])
                fw.op('dve', lambda v, ps=ps, m=m, tb=tb: v.scalar_tensor_tensor(
                    out=X[:, m, tbs(tb)], in0=PS[ps][:], scalar=GT8[:, m:m + 1], in1=X[:, m, tbs(tb)],
                    op0=ALU.mult, op1=ALU.add),
                    reads=[('PS', ps), 'GT8', ('X', m, tb)], writes=[('X', m, tb)])

        def set_gate(l, part, scale):
            fw.op('dve', lambda v: v.tensor_scalar(out=GT8[:], in0=COND[:, l, part * 8:part * 8 + 8], scalar1=scale,
                                                  scalar2=None, op0=ALU.mult),
                  reads=[('COND', l)], writes=['GT8'])

        def ffn(l, f, pre=None):
            Wa = w13d[f][l].rearrange("(k p) n -> p k n", p=128)
            Wb = w2d[f][l].rearrange("(k p) n -> p k n", p=128)
            set_gate(l, 2 if f == 0 else 8, 0.5)
            prev = None
            for j in range(int(os.environ.get('FFN_NJ', '11'))):
                b = nxt('w8', 2)
                fw.dma('pool', [lambda e, b=b, j=j: e.dma_start(out=W8[b][:, :, 0:256], in_=Wa[:, :, j * 256:(j + 1) * 256]),
                                lambda e, b=b, j=j: e.dma_start(out=W8[b][:, :, 256:512],
                                                                in_=Wa[:, :, 2816 + j * 256:2816 + (j + 1) * 256])],
                       writes=[('W8', b)])
                b2 = nxt('w2', 2)
                fw.dma('pool', lambda e, b2=b2, j=j: e.dma_start(out=W2[b2][:], in_=Wb[:, 2 * j:2 * j + 2, :]),
                       writes=[('W2', b2)])
                ai = j % 2
                for tb in range(4):
                    for fn_ in (pre or {}).get(4 * j + tb, ()):
                        fn_()
                    if f == 1 and l + 1 < L and 4 * j + tb < 36:
                        cond_piece(l + 1, 4 * j + tb)
                    pss = []
                    for q in range(4):
                        ps = psn()
                        pss.append(ps)
                        fw.ops('pe', [mm(PS[ps][:, :], lhsT=W8[b][:, k, q * 128:(q + 1) * 128], rhs=H[:, k, tbs(tb)],
                                         start=(k == 0), stop=(k == 7)) for k in range(8)],
                               reads=[('W8', b)] + [('H', k, tb) for k in range(8)], writes=[('PS', ps)])
                    for c in range(2):
                        t = tmpf()
                        fw.op('act', lambda a, t=t, p=pss[2 + c]: a.activation(out=T[t][:], in_=PS[p][:], func=AF.Silu),
                              reads=[('PS', pss[2 + c])], writes=[('T', t)])
                        fw.op('dve', lambda v, t=t, p=pss[c], c=c, tb=tb, ai=ai: v.tensor_tensor(
                            out=A[ai][:, c, tbs(tb)], in0=PS[p][:], in1=T[t][:], op=ALU.mult),
                            reads=[('PS', pss[c]), ('T', t)], writes=[('A', ai, c, tb)])
                    if prev is not None:
                        emit_w2(*prev)
                    prev = (b2, ai, tb)
            emit_w2(*prev)
            if f == 1 and l + 1 < L:
                cond_finish(l + 1)

        def load_w8(Win, pieces):
            b = nxt('w8', 2)
            fw.dma('pool', [lambda e, b=b, d=d, s=s, n=n: e.dma_start(out=W8[b][:, :, d:d + n], in_=Win[:, :, s:s + n])
                            for (d, s, n) in pieces], writes=[('W8', b)])
            return b

        def proj_chunk(b, wcol, tb):
            ps = psn()
            fw.ops('pe', [mm(PS[ps][:, :], lhsT=W8[b][:, k, wcol:wcol + 128], rhs=H[:, k, tbs(tb)],
                             start=(k == 0), stop=(k == 7)) for k in range(8)],
                   reads=[('W8', b)] + [('H', k, tb) for k in range(8)], writes=[('PS', ps)])
            return ps

        def proj_tm(Win, vcol, gcol, ng, pre=None):
            pieces = []
            nv = 0
            if vcol is not None:
                pieces.append((0, vcol, 256))
                nv = 256
            if ng:
                pieces.append((nv, gcol, ng))
            b = load_w8(Win, pieces)
            n = nv + ng
            for i in range(16):
                for fn_ in (pre or {}).get(i, ()):
                    fn_()
                ps = psn()
                fw.ops('pe', [mm(PS[ps][:, 0:n], lhsT=H[:, k, i * 128:(i + 1) * 128], rhs=W8[b][:, k, 0:n],
                                 start=(k == 0), stop=(k == 7)) for k in range(8)],
                       reads=[('W8', b)] + [('H', k, i // 4) for k in range(8)], writes=[('PS', ps)])
                if nv:
                    for u in range(2):
                        fw.op('act', lambda a, ps=ps, i=i, u=u: a.activation(
                            out=v_pair(i, u),
                            in_=PS[ps][:, 128 * u:128 * u + 128].rearrange("p (a b) -> p a b", b=64), func=AF.Copy),
                            reads=[('PS', ps)], writes=[('V', i)])
                if ng:
                    fw.op('act', lambda a, ps=ps, i=i: a.activation(out=GPT[:, i, 0:ng], in_=PS[ps][:, nv:nv + ng], func=AF.Copy),
                          reads=[('PS', ps)], writes=['GPT'])

        def bcast4(c0):
            return PP[:, c0:c0 + 4].unsqueeze(1).to_broadcast([128, 16, 4])

        def softplus_parts(z, tmp_a, out_l):
            fw.op('act', lambda a: a.activation(out=G[tmp_a][:], in_=G[z][:], func=AF.Abs),
                  reads=[('G', z)], writes=[('G', tmp_a)])
            fw.op('act', lambda a: a.activation(out=G[tmp_a][:], in_=G[tmp_a][:], func=AF.Exp, scale=-1.0),
                  reads=[('G', tmp_a)], writes=[('G', tmp_a)])
            fw.op('act', lambda a: a.activation(out=G[out_l][:], in_=G[tmp_a][:], func=AF.Ln, bias=ONE1[:, 0:1], scale=1.0),
                  reads=[('G', tmp_a), 'ONE1'], writes=[('G', out_l)])

        ONE1 = fw.sb("ONE1", [128, 1], F32)
        fw.op('pool', lambda g: g.memset(ONE1[:], 1.0), writes=['ONE1'])

        def cumsum(src, dst):
            p1 = psn()
            p2 = psn()
            flat = G[src][:].rearrange("p a b -> p (a b)")
            fw.ops('pe', [mm(PS[p1][:, 0:64], lhsT=TRI, rhs=flat, start=True, stop=True)],
                   reads=[('G', src), 'CF'], writes=[('PS', p1)])
            fw.ops('pe', [mm(PS[p2][:, 0:64], lhsT=ONESF, rhs=flat, start=True, stop=True)],
                   reads=[('G', src), 'CF'], writes=[('PS', p2)])
            tot = PS[p2][:, 0:64].rearrange("p (a b) -> p a b", b=4)
            fw.op('dve', lambda v: v.memset(PRE[:, 0, :], 0.0), writes=['PRE'])
            for i in range(1, 16):
                fw.op('dve', lambda v, i=i: v.tensor_tensor(out=PRE[:, i, :], in0=PRE[:, i - 1, :], in1=tot[:, i - 1, :],
                                                           op=ALU.add),
                      reads=['PRE', ('PS', p2)], writes=['PRE'])
            fw.op('dve', lambda v: v.tensor_tensor(out=G[dst][:], in0=PS[p1][:, 0:64].rearrange("p (a b) -> p a b", b=4),
                                                  in1=PRE[:], op=ALU.add),
                  reads=['PRE', ('PS', p1)], writes=[('G', dst)])

        SB3 = [fw.sb("SB3_%d" % i, [128, 16, 4], BF16) for i in range(3)]

        def build_aug(val, t1, t2):
            spl_keys = [('SPL', i) for i in range(16)]
            fw.op('dve', lambda v: v.tensor_copy(out=SB3[0][:], in_=G[val][:]), reads=[('G', val)], writes=[('SB3', 0)])
            fw.op('dve', lambda v: v.tensor_tensor(out=G[t1][:], in0=G[val][:], in1=SB3[0][:], op=ALU.subtract),
                  reads=[('G', val), ('SB3', 0)], writes=[('G', t1)])
            fw.op('dve', lambda v: v.tensor_copy(out=SB3[1][:], in_=G[t1][:]), reads=[('G', t1)], writes=[('SB3', 1)])
            fw.op('dve', lambda v: v.tensor_tensor(out=G[t2][:], in0=G[t1][:], in1=SB3[1][:], op=ALU.subtract),
                  reads=[('G', t1), ('SB3', 1)], writes=[('G', t2)])
            fw.op('dve', lambda v: v.tensor_copy(out=SB3[2][:], in_=G[t2][:]), reads=[('G', t2)], writes=[('SB3', 2)])
            v4 = SPL[:].rearrange("p a (h r) -> p a h r", r=64)
            for j in range(2):
                for r in range(3):
                    fw.op('dve', lambda v, j=j, r=r: v.tensor_copy(out=v4[:, :, :, r], in_=SB3[r][:, :, 2 * j:2 * j + 2]),
                          reads=[('SB3', r)], writes=spl_keys)
                for tb in range(4):
                    half = tb % 2
                    fw.ops('pe', [lambda t, i=i, half=half: t.transpose(
                        out=PSB[:, half * 512 + (i % 4) * 128:half * 512 + (i % 4 + 1) * 128], in_=SPL[:, i, :], identity=IDENT)
                        for i in range(4 * tb, 4 * tb + 4)],
                        reads=spl_keys + ['CB'], writes=[('PS', 7)])
                    fw.op('act', lambda a, tb=tb, half=half, j=j: a.activation(out=AUG[:, j, tbs(tb)],
                                                                               in_=PSB[:, half * 512:(half + 1) * 512],
                                                                               func=AF.Copy),
                          reads=[('PS', 7)], writes=[('AUG', j, tb)])

        def attention(mode, qk, vfn, M, post, pre_tb=None, share_qk=False):
            LOOK = 2
            if share_qk:
                tasks = [(tb, 2 * g + u, sc) for tb in range(4) for g in range(2) for sc in range(4 * tb + 4)
                         for u in range(2)]
            else:
                tasks = [(tb, h, sc) for tb in range(4) for h in range(4) for sc in range(4 * tb + 4)]
            acc = {}
            shared = {}

            def phase_a(tb, h, sc):
                qa, ka = qk(h)
                pq = 64 * (h % 2)
                slab = h // 2
                d = sc - 4 * tb
                scs = slice(sc * 128, (sc + 1) * 128)
                trow = lambda ps, first, last: mm(PS[ps][:, :], lhsT=CB[pq:pq + 64, CB_ONES:CB_ONES + 128],
                                                  rhs=AUG[pq:pq + 64, slab, tbs(tb)], start=first, stop=last)

                def maskmms(ps):
                    c1 = CB_MASK + 448 - 128 * d
                    c2 = c1 - 64
                    return [mm(PS[ps][:, :], lhsT=CB[pq:pq + 64, CB_SEL1:CB_SEL1 + 128], rhs=CB[pq:pq + 64, c1:c1 + 512],
                               start=False, stop=False),
                            mm(PS[ps][:, :], lhsT=CB[pq:pq + 64, CB_SEL2:CB_SEL2 + 128], rhs=CB[pq:pq + 64, c2:c2 + 512],
                               start=False, stop=True)]
                rd_qk = [qk_key(h, 'q', tb), qk_key(h, 'k', sc // 4)]
                rd_aug = [('AUG', slab, tb)]
                sbias = G[5][:, sc, h:h + 1]
                pt = ptn()
                if mode == 'softmax':
                    ps = psn()
                    fns = [mm(PS[ps][:, :], lhsT=ka[:, scs], rhs=qa[:, tbs(tb)], start=True, stop=False),
                           trow(ps, False, d < 0)]
                    if d >= 0:
                        fns += maskmms(ps)
                    fw.ops('pe', fns, reads=rd_qk + rd_aug + ['CB'], writes=[('PS', ps)])
                    fw.op('act', lambda a, ps=ps, pt=pt: a.activation(out=PTB[pt][:], in_=PS[ps][:], func=AF.Exp,
                                                                      bias=sbias, scale=1.0),
                          reads=[('PS', ps), ('G', 5)], writes=[('PT', pt)])
                else:
                    if share_qk and h % 2 == 1:
                        ps = shared['ps']
                    else:
                        ps = psn()
                        shared['ps'] = ps
                        fw.ops('pe', [mm(PS[ps][:, :], lhsT=ka[:, scs], rhs=qa[:, tbs(tb)], start=True, stop=True)],
                               reads=rd_qk, writes=[('PS', ps)])
                    pl = psn()
                    fns = [trow(pl, True, d < 0)]
                    if d >= 0:
                        fns += maskmms(pl)
                    fw.ops('pe', fns, reads=rd_aug + ['CB'], writes=[('PS', pl)])
                    e = tmpf()
                    fw.op('act', lambda a, pl=pl, e=e: a.activation(out=T[e][:], in_=PS[pl][:], func=AF.Exp,
                                                                    bias=sbias, scale=1.0),
                          reads=[('PS', pl), ('G', 5)], writes=[('T', e)])
                    fw.op('dve', lambda v, ps=ps, e=e, pt=pt: v.tensor_tensor(out=PTB[pt][:], in0=PS[ps][:],
                                                                            in1=T[e][:], op=ALU.mult),
                          reads=[('PS', ps), ('T', e)], writes=[('PT', pt)])
                return pt

            def phase_b(tb, h, sc, pt):
                nsc = 4 * tb + 4
                if sc == 0:
                    acc[h] = pon()
                po = acc[h]
                fw.ops('pe', [mm(PS[po][:, :], lhsT=vfn(h, sc), rhs=PTB[pt][:], start=(sc == 0), stop=(sc == nsc - 1))],
                       reads=[('PT', pt), ('V', sc)], writes=[('PS', po)])
                if sc == nsc - 1:
                    cont = post(h, tb, po)
                    if cont is not None:
                        deferred.append((it[0] + 2, cont))

            pend = []
            deferred = []
            it = [0]
            for i0 in range(0, len(tasks), LOOK):
                for (tb, h, sc) in tasks[i0:i0 + LOOK]:
                    pend.append((tb, h, sc, phase_a(tb, h, sc)))
                it[0] += 1
                while deferred and deferred[0][0] <= it[0]:
                    deferred.pop(0)[1]()
                while len(pend) > LOOK:
                    phase_b(*pend.pop(0))
            while pend:
                phase_b(*pend.pop(0))
            while deferred:
                deferred.pop(0)[1]()

        def qk_key(h, which, blk):
            return ('QK', h, which, blk)

        def head_norm_gate(l, h, tb, tsrc, gcol):
            pb = 64 * (h % 2)
            c = h // 2
            fw.op('act', lambda a: a.activation(out=A[2][pb:pb + 64, c, tbs(tb)], in_=tsrc, func=AF.Square),
                  reads=['ROPE'], writes=[('A', 2, c, tb)])

            def cont():
                head_norm_gate2(l, h, tb, tsrc, gcol)
            return cont

        def head_norm_gate2(l, h, tb, tsrc, gcol):
            pb = 64 * (h % 2)
            c = h // 2
            pn = psn()
            fw.ops('pe', [mm(PS[pn][0:64, :], lhsT=CB[pb:pb + 64, CB_ONES:CB_ONES + 64], rhs=A[2][pb:pb + 64, c, tbs(tb)],
                             start=True, stop=True)], reads=[('A', 2, c, tb), 'CB'], writes=[('PS', pn)])
            t2 = tmpf()
            fw.op('act', lambda a: a.activation(out=T[t2][pb:pb + 64, :], in_=PS[pn][0:64, :], func=AF.Sqrt,
                                                scale=1.0 / 64.0, bias=EPSB[pb:pb + 64, 0:1]),
                  reads=[('PS', pn), 'EPSB'], writes=[('T', t2)])
            fw.op('dve', lambda v: v.reciprocal(out=T[t2][pb:pb + 64, :], in_=T[t2][pb:pb + 64, :]),
                  reads=[('T', t2)], writes=[('T', t2)])
            fw.op('dve', lambda v: v.scalar_tensor_tensor(out=tsrc, in0=tsrc,
                                                         scalar=PP[pb:pb + 64, gcol + c:gcol + c + 1],
                                                         in1=T[t2][pb:pb + 64, :], op0=ALU.mult, op1=ALU.mult),
                  reads=['ROPE', ('T', t2), 'PP'], writes=['ROPE'])
            fw.op('dve', lambda v: v.tensor_tensor(out=A[2][pb:pb + 64, c, tbs(tb)], in0=tsrc,
                                                  in1=A[3][pb:pb + 64, c, tbs(tb)], op=ALU.mult),
                  reads=['ROPE', ('A', 3, c, tb)], writes=[('A', 2, c, tb)])

        def conv_silu(l, ps, wcol, bcol, cc, tb, out_ap, wkeys):
            if tb == 0:
                fw.op('dve', lambda v: v.memset(CS[:, 0:3], 0.0), writes=['CS'])
            fw.op('act', lambda a: a.activation(out=CS[:, 3:515], in_=PS[ps][:], func=AF.Copy),
                  reads=[('PS', ps)], writes=['CS'])
            t = tmpf()
            fw.op('dve', lambda v: v.tensor_scalar(out=T[t][:], in0=CS[:, 0:512], scalar1=PP[:, wcol + cc * 4:wcol + cc * 4 + 1],
                                                  scalar2=None, op0=ALU.mult), reads=['CS', 'PP'], writes=[('T', t)])
            for j in range(1, 4):
                fw.op('dve', lambda v, j=j: v.scalar_tensor_tensor(out=T[t][:], in0=CS[:, j:j + 512],
                                                                  scalar=PP[:, wcol + cc * 4 + j:wcol + cc * 4 + j + 1],
                                                                  in1=T[t][:], op0=ALU.mult, op1=ALU.add),
                      reads=['CS', 'PP', ('T', t)], writes=[('T', t)])
            if tb < 3:
                fw.op('dve', lambda v: v.tensor_copy(out=CS[:, 0:3], in_=CS[:, 512:515]), reads=['CS'], writes=['CS'])
            fw.op('act', lambda a: a.activation(out=out_ap, in_=T[t][:], func=AF.Silu, bias=PP[:, bcol + cc:bcol + cc + 1],
                                                scale=1.0), reads=[('T', t), 'PP'], writes=wkeys)

        def out_proj(l, mi):
            Wout = w_out[l].rearrange("(k p) n -> p k n", p=128)
            b2 = nxt('w2', 2)
            fw.dma('pool', lambda e: e.dma_start(out=W2[b2][:], in_=Wout[:, 2 * mi:2 * mi + 2, :]), writes=[('W2', b2)])
            for tb in range(4):
                emit_w2(b2, 2, tb)

        def std_qk(h):
            return A[0][64 * (h % 2):64 * (h % 2) + 64, h // 2, :], A[1][64 * (h % 2):64 * (h % 2) + 64, h // 2, :]

        def qk_writes(which, c, tb):
            return [qk_key(2 * c, which, tb), qk_key(2 * c + 1, which, tb), ('A', 0 if which == 'q' else 1, c, tb)]

        def v_pair(i, u):
            base = V[:, i, 192 * u:192 * u + 64]
            return bass.AP(tensor=base.tensor, offset=base.offset, ap=[list(base.ap[0]), [128, 2], [1, 64]])

        VAUG0 = [0, 64, 192, 256]
        VCOL = [0, 128, 192, 320]

        def v_aug(h, sc):
            return V[:, sc, VAUG0[h]:VAUG0[h] + 128]

        def v_plain(h, sc):
            return V[:, sc, VCOL[h]:VCOL[h] + 64]

        def mixer(l):
            o = PPL['L'][l]
            Win = w_in[l].rearrange("(k p) n -> p k n", p=128)
            set_gate(l, 5, 1.0)
            proj_tm(Win, 512, 768, 4, pre={0: [lambda: norm_tb(l, 1, 0), lambda: norm_tb(l, 1, 1)],
                                            4: [lambda: norm_tb(l, 1, 2)], 8: [lambda: norm_tb(l, 1, 3)]})
            if stop == 'fox_g1':
                return
            fw.op('dve', lambda v: v.tensor_tensor(out=G[0][:], in0=GPT[:, :, 0:4], in1=bcast4(o['ffb']), op=ALU.add),
                  reads=['GPT', 'PP'], writes=[('G', 0)])
            softplus_parts(0, 1, 2)
            fw.op('dve', lambda v: v.scalar_tensor_tensor(out=G[3][:], in0=G[0][:], scalar=0.0, in1=G[2][:],
                                                         op0=ALU.min, op1=ALU.subtract),
                  reads=[('G', 0), ('G', 2)], writes=[('G', 3)])
            if stop == 'fox_g2':
                return
            cumsum(3, 4)
            fw.op('dve', lambda v: v.tensor_scalar(out=G[5][:], in0=G[4][:], scalar1=-1.0, scalar2=None, op0=ALU.mult),
                  reads=[('G', 4)], writes=[('G', 5)])
            if stop == 'fox_g':
                return
            build_aug(4, 0, 1)
            if stop == 'fox_a':
                return
            b = load_w8(Win, [(0, 0, 512)])
            for cc in range(4):
                for tb in range(4):
                    ps = proj_chunk(b, cc * 128, tb)
                    if cc < 2:
                        fw.op('act', lambda a, ps=ps, cc=cc, tb=tb: a.activation(out=A[0][:, cc, tbs(tb)], in_=PS[ps][:],
                                                                                func=AF.Copy, scale=0.125),
                              reads=[('PS', ps)], writes=qk_writes('q', cc, tb))
                    else:
                        fw.op('act', lambda a, ps=ps, cc=cc, tb=tb: a.activation(out=A[1][:, cc - 2, tbs(tb)], in_=PS[ps][:],
                                                                                func=AF.Copy),
                              reads=[('PS', ps)], writes=qk_writes('k', cc - 2, tb))

            def post_fox(h, tb, po):
                pb = 64 * (h % 2)
                r = tmpf()
                fw.op('dve', lambda v: v.reciprocal(out=T[r][pb:pb + 64, :], in_=PS[po][64 - pb:128 - pb, :]),
                      reads=[('PS', po)], writes=[('T', r)])
                fw.op('dve', lambda v: v.tensor_tensor(out=A[2][pb:pb + 64, h // 2, tbs(tb)], in0=PS[po][pb:pb + 64, :],
                                                      in1=T[r][pb:pb + 64, :], op=ALU.mult),
                      reads=[('PS', po), ('T', r)], writes=[('A', 2, h // 2, tb)])
            attention('softmax', std_qk, v_aug, 128, post_fox)
            out_proj(l, 0)
            if stop == 'fox':
                return
            proj_tm(Win, 1284, 1540, 8)
            fw.op('dve', lambda v: v.tensor_tensor(out=G[6][:], in0=GPT[:, :, 0:4], in1=bcast4(o['mib']), op=ALU.add),
                  reads=['GPT', 'PP'], writes=[('G', 6)])
            fw.op('dve', lambda v: v.tensor_tensor(out=G[0][:], in0=GPT[:, :, 4:8], in1=bcast4(o['mfb']), op=ALU.add),
                  reads=['GPT', 'PP'], writes=[('G', 0)])
            softplus_parts(0, 1, 2)
            fw.op('dve', lambda v: v.scalar_tensor_tensor(out=G[3][:], in0=G[0][:], scalar=0.0, in1=G[2][:],
                                                         op0=ALU.min, op1=ALU.subtract),
                  reads=[('G', 0), ('G', 2)], writes=[('G', 3)])
            cumsum(3, 4)
            fw.op('dve', lambda v: v.scalar_tensor_tensor(out=G[5][:], in0=G[6][:], scalar=math.log(0.125), in1=G[4][:],
                                                         op0=ALU.add, op1=ALU.subtract),
                  reads=[('G', 6), ('G', 4)], writes=[('G', 5)])
            build_aug(4, 0, 1)
            b = load_w8(Win, [(0, 1548, 256)])
            for cc in range(2):
                for tb in range(4):
                    ps = proj_chunk(b, cc * 128, tb)
                    fw.op('act', lambda a, ps=ps, cc=cc, tb=tb: a.activation(out=A[3][:, cc, tbs(tb)], in_=PS[ps][:],
                                                                            func=AF.Sigmoid),
                          reads=[('PS', ps)], writes=[('A', 3, cc, tb)])
            b = load_w8(Win, [(0, 772, 512)])
            for cc in range(4):
                for tb in range(4):
                    ps = proj_chunk(b, cc * 128, tb)
                    if cc < 2:
                        conv_silu(l, ps, o['mcw'], o['mcb'], cc, tb, A[0][:, cc, tbs(tb)], qk_writes('q', cc, tb))
                    else:
                        conv_silu(l, ps, o['mcw'], o['mcb'], cc, tb, A[1][:, cc - 2, tbs(tb)], qk_writes('k', cc - 2, tb))

            def post_mlstm(h, tb, po):
                pb = 64 * (h % 2)
                r = tmpf()
                fw.op('act', lambda a: a.activation(out=T[r][pb:pb + 64, :], in_=PS[po][64 - pb:128 - pb, :], func=AF.Abs),
                      reads=[('PS', po)], writes=[('T', r)])
                fw.op('dve', lambda v: v.tensor_scalar(out=T[r][pb:pb + 64, :], in0=T[r][pb:pb + 64, :], scalar1=1.0,
                                                      scalar2=None, op0=ALU.max),
                      reads=[('T', r)], writes=[('T', r)])
                fw.op('dve', lambda v: v.reciprocal(out=T[r][pb:pb + 64, :], in_=T[r][pb:pb + 64, :]),
                      reads=[('T', r)], writes=[('T', r)])
                hh = ROPE[pb:pb + 64, nxt('hs', 2), :]
                fw.op('dve', lambda v: v.tensor_tensor(out=hh, in0=PS[po][pb:pb + 64, :],
                                                      in1=T[r][pb:pb + 64, :], op=ALU.mult),
                      reads=[('PS', po), ('T', r)], writes=['ROPE'])
                return head_norm_gate(l, h, tb, hh, o['mng'])
            attention('linear', std_qk, v_aug, 128, post_mlstm)
            out_proj(l, 1)
            if stop == 'mlstm':
                return
            proj_tm(Win, 2316, 0, 0)
            fw.dma('sp', lambda e: e.dma_start(out=AUG[:], in_=rad),
                   writes=[('AUG', s_, t_) for s_ in range(2) for t_ in range(4)])
            fw.op('dve', lambda v: v.tensor_copy(out=G[5][:].rearrange("p a b -> p (a b)"), in_=CF[:, CF_RS:CF_RS + 64]),
                  reads=['CF'], writes=[('G', 5)])
            b = load_w8(Win, [(0, 2572, 256)])
            for cc in range(2):
                for tb in range(4):
                    ps = proj_chunk(b, cc * 128, tb)
                    fw.op('act', lambda a, ps=ps, cc=cc, tb=tb: a.activation(out=A[3][:, cc, tbs(tb)], in_=PS[ps][:],
                                                                            func=AF.Silu),
                          reads=[('PS', ps)], writes=[('A', 3, cc, tb)])
            for which, base in (('q', 1804), ('k', 2060)):
                pieces = [(0, base, 256)]
                for hh in range(4):
                    pieces.append((256 + 64 * hh, base + 64 * hh + 32, 32))
                    pieces.append((256 + 64 * hh + 32, base + 64 * hh, 32))
                b = load_w8(Win, pieces)
                dst = A[0] if which == 'q' else A[1]
                for tb in range(4):
                    fw.dma('sp', [lambda e, tb=tb: e.dma_start(out=ROPE[:, 0, :], in_=cfd[:, CF_COS + tb * 512:CF_COS + (tb + 1) * 512]),
                                  lambda e, tb=tb: e.dma_start(out=ROPE[:, 1, :], in_=cfd[:, CF_SIN + tb * 512:CF_SIN + (tb + 1) * 512])],
                           writes=['ROPE'])
                    for cc in range(2):
                        p1 = proj_chunk(b, cc * 128, tb)
                        p2 = proj_chunk(b, 256 + cc * 128, tb)
                        t1 = tmpf()
                        t2 = tmpf()
                        fw.op('dve', lambda v, p1=p1, t1=t1: v.tensor_tensor(out=T[t1][:], in0=PS[p1][:], in1=ROPE[:, 0, :],
                                                                             op=ALU.mult),
                              reads=[('PS', p1), 'ROPE'], writes=[('T', t1)])
                        fw.op('dve', lambda v, p2=p2, t2=t2: v.tensor_tensor(out=T[t2][:], in0=PS[p2][:], in1=ROPE[:, 1, :],
                                                                             op=ALU.mult),
                              reads=[('PS', p2), 'ROPE'], writes=[('T', t2)])
                        fw.op('dve', lambda v, t1=t1, t2=t2, cc=cc, tb=tb, dst=dst: v.tensor_tensor(
                            out=dst[:, cc, tbs(tb)], in0=T[t1][:], in1=T[t2][:], op=ALU.add),
                            reads=[('T', t1), ('T', t2)], writes=qk_writes(which, cc, tb))

            def post_ret(h, tb, po):
                pb = 64 * (h % 2)
                hh = ROPE[pb:pb + 64, nxt('hs', 2), :]
                fw.op('act', lambda a: a.activation(out=hh, in_=PS[po][pb:pb + 64, :], func=AF.Copy),
                      reads=[('PS', po)], writes=['ROPE'])
                return head_norm_gate(l, h, tb, hh, o['rng'])
            attention('linear', std_qk, v_aug, 128, post_ret)
            out_proj(l, 2)
            if stop == 'ret':
                return
            proj_tm(Win, None, 3596, 4)
            fw.op('dve', lambda v: v.tensor_tensor(out=G[0][:], in0=GPT[:, :, 0:4], in1=bcast4(o['sdtb']), op=ALU.add),
                  reads=['GPT', 'PP'], writes=[('G', 0)])
            softplus_parts(0, 1, 2)
            fw.op('dve', lambda v: v.scalar_tensor_tensor(out=G[3][:], in0=G[0][:], scalar=0.0, in1=G[2][:],
                                                         op0=ALU.max, op1=ALU.add),
                  reads=[('G', 0), ('G', 2)], writes=[('G', 3)])
            fw.op('act', lambda a: a.activation(out=AEXP[:], in_=PP[:, o['sAl']:o['sAl'] + 4], func=AF.Exp),
                  reads=['PP'], writes=['AEXP'])
            fw.op('dve', lambda v: v.scalar_tensor_tensor(out=G[6][:], in0=G[3][:], scalar=-1.0,
                                                         in1=AEXP[:].unsqueeze(1).to_broadcast([128, 16, 4]),
                                                         op0=ALU.mult, op1=ALU.mult),
                  reads=[('G', 3), 'AEXP'], writes=[('G', 6)])
            cumsum(6, 4)
            fw.op('act', lambda a: a.activation(out=G[2][:], in_=G[3][:], func=AF.Ln), reads=[('G', 3)], writes=[('G', 2)])
            fw.op('dve', lambda v: v.tensor_tensor(out=G[5][:], in0=G[2][:], in1=G[4][:], op=ALU.subtract),
                  reads=[('G', 2), ('G', 4)], writes=[('G', 5)])
            build_aug(4, 0, 1)
            if stop == 'fox_ssd_g':
                return
            b = load_w8(Win, [(0, 2828, 256)])
            for cc in range(2):
                for tb in range(4):
                    ps = proj_chunk(b, cc * 128, tb)
                    fw.op('act', lambda a, ps=ps, cc=cc, tb=tb: a.activation(out=A[3][:, cc, tbs(tb)], in_=PS[ps][:],
                                                                            func=AF.Silu),
                          reads=[('PS', ps)], writes=[('A', 3, cc, tb)])
            b = load_w8(Win, [(0, 3084, 512)])
            dsts = [(0, 1), (1, 1), (1, 0), (0, 0)]
            for cc in range(4):
                ai, ac = dsts[cc]
                for tb in range(4):
                    ps = proj_chunk(b, cc * 128, tb)
                    wk = [('A', ai, ac, tb)]
                    if cc == 2:
                        wk += [qk_key(hh, 'k', tb) for hh in range(4)]
                    if cc == 3:
                        wk += [qk_key(hh, 'q', tb) for hh in range(4)]
                    conv_silu(l, ps, o['scw'], o['scb'], cc, tb, A[ai][:, ac, tbs(tb)], wk)
            for i in range(16):
                half = i % 2
                fw.ops('pe', [lambda t, i=i, c=c, half=half: t.transpose(
                    out=PSB[:, half * 512 + c * 128:half * 512 + (c + 1) * 128],
                    in_=A[c][:, 1, i * 128:(i + 1) * 128], identity=IDENT) for c in range(2)],
                    reads=[('A', 0, 1, i // 4), ('A', 1, 1, i // 4), 'CB'], writes=[('PS', 7)])
                for u in range(2):
                    fw.op('act', lambda a, i=i, half=half, u=u: a.activation(
                        out=v_pair(i, u),
                        in_=PSB[:, half * 512 + 128 * u:half * 512 + 128 * u + 128].rearrange("p (a b) -> p a b", b=64),
                        func=AF.Copy), reads=[('PS', 7)], writes=[('V', i)])

            if stop == 'fox_ssd_p':
                return

            def ssd_qk(h):
                g = h // 2
                return A[0][64 * g:64 * g + 64, 0, :], A[1][64 * g:64 * g + 64, 0, :]
            ssd_tiles = {}

            def pre_ssd(tb):
                pass

            def post_ssd(h, tb, po):
                if os.environ.get('SKIP_SSD_POST') == '1':
                    return
                pb = 64 * (h % 2)
                c = h // 2
                xh = A[c][pb:pb + 64, 1, tbs(tb)]
                fw.op('dve', lambda v: v.scalar_tensor_tensor(out=ROPE[pb:pb + 64, c, :], in0=xh,
                                                             scalar=PP[pb:pb + 64, o['sD'] + c:o['sD'] + c + 1],
                                                             in1=PS[po][pb:pb + 64, :], op0=ALU.mult, op1=ALU.add),
                      reads=[('A', c, 1, tb), 'PP', ('PS', po)], writes=['ROPE'])
                fw.op('dve', lambda v: v.tensor_tensor(out=ROPE[pb:pb + 64, c, :], in0=ROPE[pb:pb + 64, c, :],
                                                      in1=A[3][pb:pb + 64, c, tbs(tb)], op=ALU.mult),
                      reads=['ROPE', ('A', 3, c, tb)], writes=['ROPE'])
                if h == 3 and os.environ.get('SKIP_SSD_POST') != '2':
                    y0, y1 = 0, 1
                    ykeys = ['ROPE']
                    for c2, yy in enumerate((y0, y1)):
                        fw.op('act', lambda a, yy=yy, c2=c2: a.activation(out=A[2][:, c2, tbs(tb)], in_=ROPE[:, yy, :],
                                                                          func=AF.Square),
                              reads=ykeys, writes=[('A', 2, c2, tb)])
                    return lambda: post_ssd2(tb, ykeys, y0, y1)

            def post_ssd2(tb, ykeys, y0, y1):
                if True:
                    pn = psn()
                    fw.ops('pe', [mm(PS[pn][:, :], lhsT=ONESB, rhs=A[2][:, c2, tbs(tb)], start=(c2 == 0), stop=(c2 == 1))
                                  for c2 in range(2)], reads=[('A', 2, 0, tb), ('A', 2, 1, tb), 'CB'], writes=[('PS', pn)])
                    r = tmpf()
                    fw.op('act', lambda a: a.activation(out=T[r][:], in_=PS[pn][:], func=AF.Sqrt, scale=1.0 / 256.0,
                                                        bias=EPSB[:, 0:1]), reads=[('PS', pn), 'EPSB'], writes=[('T', r)])
                    fw.op('dve', lambda v: v.reciprocal(out=T[r][:], in_=T[r][:]), reads=[('T', r)], writes=[('T', r)])
                    for c2, yy in enumerate((y0, y1)):
                        fw.op('dve', lambda v, c2=c2, yy=yy: v.scalar_tensor_tensor(
                            out=A[2][:, c2, tbs(tb)], in0=ROPE[:, yy, :], scalar=PP[:, o['sng'] + c2:o['sng'] + c2 + 1],
                            in1=T[r][:], op0=ALU.mult, op1=ALU.mult),
                            reads=ykeys + [('T', r), 'PP'], writes=[('A', 2, c2, tb)])
            attention('linear', ssd_qk, v_aug, 128, post_ssd, share_qk=True)
            out_proj(l, 3)

        AEXP = fw.sb("AEXP", [128, 4], F32)

        done = False
        for l in range(L):
            if stop == 'h0':
                norm_mod(l, 0)
                break
            norm_prep(l, 0)
            ffn(l, 0, pre={0: [lambda l=l: norm_tb(l, 0, 0), lambda l=l: norm_tb(l, 0, 1)],
                           1: [lambda l=l: norm_tb(l, 0, 2)], 2: [lambda l=l: norm_tb(l, 0, 3)]})
            if stop == 'ffn1':
                break
            norm_prep(l, 1)
            mixer(l)
            if stop is not None and (stop in ('fox', 'mlstm', 'ret', 'mix') or stop.startswith('fox_')):
                break
            norm_prep(l, 2)
            ffn(l, 1, pre={0: [lambda l=l: norm_tb(l, 2, 0), lambda l=l: norm_tb(l, 2, 1)],
                           1: [lambda l=l: norm_tb(l, 2, 2)], 2: [lambda l=l: norm_tb(l, 2, 3)]})
        out_toks = []
        if stop == 'h0':
            for c in range(8):
                for tb in range(4):
                    t = tmpf()
                    fw.op('dve', lambda v, c=c, tb=tb, t=t: v.tensor_copy(out=T[t][:], in_=H[:, c, tbs(tb)]),
                          reads=[('H', c, tb)], writes=[('T', t)])
                    out_toks.append(fw.dma('sp', lambda e, c=c, tb=tb, t=t: e.dma_start(
                        out=yT[c * 128:(c + 1) * 128, tbs(tb)], in_=T[t][:]), reads=[('T', t)]))
        elif stop is not None:
            for c in range(8):
                out_toks.append(fw.dma('sp', lambda e, c=c: e.dma_start(out=yT[c * 128:(c + 1) * 128, :], in_=X[:, c, :]),
                                       reads=[('X', c, tb) for tb in range(4)]))
        else:
            SQv = A[0][:].rearrange("p a (b n) -> p (a b) n", n=512)
            a0keys = [('A', 0, c, t) for c in range(2) for t in range(4)]
            for tb in range(4):
                fw.op('act', lambda a, tb=tb: a.activation(out=SQv, in_=X[:, :, tbs(tb)], func=AF.Square),
                      reads=[('X', c, tb) for c in range(8)], writes=a0keys)
                ps = psn()
                fw.ops('pe', [mm(PS[ps][:, :], lhsT=ONESB, rhs=SQv[:, c, :], start=(c == 0), stop=(c == 7))
                              for c in range(8)], reads=a0keys + ['CB'], writes=[('PS', ps)])
                fw.op('act', lambda a, ps=ps: a.activation(out=RSTD, in_=PS[ps][:], func=AF.Sqrt,
                                                           scale=1.0 / 1024.0, bias=EPSB[:, 0:1]),
                      reads=[('PS', ps), 'EPSB'], writes=['CS'])
                fw.op('dve', lambda v: v.reciprocal(out=RSTD, in_=RSTD), reads=['CS'], writes=['CS'])
                for c in range(8):
                    fw.op('dve', lambda v, c=c, tb=tb: v.scalar_tensor_tensor(
                        out=X[:, c, tbs(tb)], in0=X[:, c, tbs(tb)], scalar=PP[:, PPL['fg'] + c:PPL['fg'] + c + 1],
                        in1=RSTD, op0=ALU.mult, op1=ALU.mult),
                        reads=[('X', c, tb), 'CS', 'PP'], writes=[('X', c, tb)])
            for c in range(8):
                out_toks.append(fw.dma('sp', lambda e, c=c: e.dma_start(out=yT[c * 128:(c + 1) * 128, :], in_=X[:, c, :]),
                                       reads=[('X', c, tb) for tb in range(4)]))
        fw.wait_all('sp', out_toks)
        fw.emit()
        print("kernel build: instructions", fw.n_ins, "waits", fw.n_wait, {e: fw.cnt[e] for e in ENGS}, flush=True)
    return nc


_CONSTS = None


def make_in_maps(inp, cores, NLW=4):
    global _CONSTS
    if _CONSTS is None:
        _CONSTS = make_consts()
    cb, cf, ra = _CONSTS
    f32 = lambda a: np.ascontiguousarray(np.asarray(a, np.float32))
    shared = {k: f32(inp[k][:NLW]) for k in ('ada_w', 'ffn1_w13', 'ffn1_w2', 'ffn2_w13', 'ffn2_w2', 'w_in', 'w_out')}
    maps = []
    for b in cores:
        m = dict(shared)
        m['xT'] = np.ascontiguousarray(np.asarray(inp['x'][b], np.float32).T)
        m['pp'] = pack_pp(inp, b)
        m['cb'] = cb
        m['cf'] = cf
        m['retaug'] = ra
        maps.append(m)
    return maps


def kernel(**inputs):
    inp = {k: np.asarray(v) for k, v in inputs.items()}
    nc = build(L=4)
    maps = make_in_maps(inp, list(range(8)))
    res = run_bass_kernel_spmd(nc, maps, core_ids=list(range(8)))
    out = np.stack([np.ascontiguousarray(res.results[b]['yT'].T) for b in range(8)], axis=0)
    return out.astype(np.float32)
```
